# Optimizing a Trainium2 kernel written in Bass

```python
import jax, jax.numpy as jnp
from jax import lax
import numpy as np

D_MODEL = 1024
BATCH = 2
SEQ = 8192
DEPTH = 1

HEAD_DIM = 64
N_Q_HEADS = 8
N_KV_HEADS = 2
Q_REP = N_Q_HEADS // N_KV_HEADS
ATTN_WIDTH = N_Q_HEADS * HEAD_DIM
KV_WIDTH = N_KV_HEADS * HEAD_DIM
WINDOW = 128
ATTN_BLOCK = 128
N_GMLP_GROUPS = 8
GMLP_WIDTH = D_MODEL - ATTN_WIDTH
GMLP_GROUP_DIM = GMLP_WIDTH // N_GMLP_GROUPS
CHUNK = 128
MIX_WIDTH = ATTN_WIDTH + GMLP_WIDTH
IN_WIDTH = ATTN_WIDTH + 2 * KV_WIDTH + 2 * GMLP_WIDTH
N_EXPERTS = 32
TOP_K = 4
D_FF = D_MODEL
SWIGLU_LIMIT = 7.0
SWIGLU_ALPHA = 1.702
MOE_BLOCK = 128
LN_EPS = 1e-5
DEEPNORM_ALPHA = (2.0 * DEPTH) ** 0.25
DEEPNORM_BETA = (8.0 * DEPTH) ** -0.25
NEG_INF = -1e30

kernel_name = "hybrid_swa_sink_gmlp_moe_deepnorm"


def layer_norm(x, g, b):
    xf = x.astype(jnp.float32)
    mu = jnp.mean(xf, axis=-1, keepdims=True)
    xc = xf - mu
    var = jnp.mean(xc * xc, axis=-1, keepdims=True)
    return (xc * lax.rsqrt(var + LN_EPS)).astype(x.dtype) * g + b


def sliding_window_attention(q, k, v, sinks):
    B, S = q.shape[:2]
    nb = S // ATTN_BLOCK
    q = q.reshape(B, nb, ATTN_BLOCK, N_KV_HEADS, Q_REP, HEAD_DIM)
    k = k.reshape(B, nb, ATTN_BLOCK, N_KV_HEADS, HEAD_DIM)
    v = v.reshape(B, nb, ATTN_BLOCK, N_KV_HEADS, HEAD_DIM)

    def band(t):
        prev = jnp.pad(t[:, :-1], ((0, 0), (1, 0), (0, 0), (0, 0), (0, 0)))
        return jnp.concatenate([prev, t], axis=2)

    kb, vb = band(k), band(v)
    scores = jnp.einsum('bnqgrd,bnkgd->bngrqk', q, kb).astype(jnp.float32) * (HEAD_DIM ** -0.5)
    t_idx = jnp.arange(ATTN_BLOCK)[:, None]
    s_idx = jnp.arange(2 * ATTN_BLOCK)[None, :]
    diff = t_idx + ATTN_BLOCK - s_idx
    in_band = (diff >= 0) & (diff < WINDOW)
    blk = jnp.arange(nb)[:, None, None]
    valid = in_band[None] & ((blk * ATTN_BLOCK - ATTN_BLOCK + s_idx[None]) >= 0)
    scores = jnp.where(valid[None, :, None, None], scores, NEG_INF)
    sink = sinks.astype(jnp.float32).reshape(N_KV_HEADS, Q_REP)[None, None, :, :, None, None]
    sink = jnp.broadcast_to(sink, scores.shape[:-1] + (1,))
    probs = jax.nn.softmax(jnp.concatenate([scores, sink], axis=-1), axis=-1)[..., :-1]
    out = jnp.einsum('bngrqk,bnkgd->bnqgrd', probs.astype(v.dtype), vb)
    return out.reshape(B, S, ATTN_WIDTH)


def chunked_spatial_gating(u, g, ln_g, ln_b, w_s, b_s):
    B, S = u.shape[:2]
    nc = S // CHUNK
    u = jax.nn.gelu(u)
    g = jax.nn.gelu(g).reshape(B, S, N_GMLP_GROUPS, GMLP_GROUP_DIM)
    g = layer_norm(g, ln_g.reshape(N_GMLP_GROUPS, GMLP_GROUP_DIM), ln_b.reshape(N_GMLP_GROUPS, GMLP_GROUP_DIM))
    g = g.reshape(B, nc, CHUNK, N_GMLP_GROUPS, GMLP_GROUP_DIM)
    causal = jnp.tril(jnp.ones((CHUNK, CHUNK), dtype=bool))
    w = jnp.where(causal[None], w_s, jnp.zeros_like(w_s))
    mixed = jnp.einsum('gts,bcsgd->bctgd', w, g) + b_s.T[None, None, :, :, None]
    return u * mixed.reshape(B, S, GMLP_WIDTH)


def token_mixer(x, w_in, b_in, sinks, ln_v_g, ln_v_b, w_spatial, b_spatial, w_out, b_out):
    proj = x @ w_in + b_in
    o1 = ATTN_WIDTH
    o2 = o1 + KV_WIDTH
    o3 = o2 + KV_WIDTH
    o4 = o3 + GMLP_WIDTH
    q, k, v = proj[..., :o1], proj[..., o1:o2], proj[..., o2:o3]
    u, g = proj[..., o3:o4], proj[..., o4:]
    attn = sliding_window_attention(q, k, v, sinks)
    sgu = chunked_spatial_gating(u, g, ln_v_g, ln_v_b, w_spatial, b_spatial)
    return jnp.concatenate([attn, sgu], axis=-1) @ w_out + b_out


def routed_experts(x, w_router, b_router, w_gate, b_gate, w_up, b_up, w_down, b_down):
    B, S, D = x.shape
    T = B * S
    TK = T * TOP_K
    xt = x.reshape(T, D)
    logits = (xt @ w_router + b_router).astype(jnp.float32)
    top_vals, top_idx = lax.top_k(logits, TOP_K)
    gates = jax.nn.softmax(top_vals, axis=-1).astype(x.dtype)
    flat_e = top_idx.reshape(-1)
    flat_tok = jnp.arange(TK, dtype=jnp.int32) // TOP_K
    order = jnp.argsort(flat_e)
    sorted_e = flat_e[order]
    counts = jnp.bincount(flat_e, length=N_EXPERTS)
    start = jnp.cumsum(counts) - counts
    padded = (counts + MOE_BLOCK - 1) // MOE_BLOCK * MOE_BLOCK
    padded_end = jnp.cumsum(padded)
    padded_start = padded_end - padded
    dest = padded_start[sorted_e] + jnp.arange(TK, dtype=jnp.int32) - start[sorted_e]
    n_blocks = TK // MOE_BLOCK + N_EXPERTS
    n_rows = n_blocks * MOE_BLOCK
    row_tok = jnp.full((n_rows,), T, dtype=jnp.int32).at[dest].set(flat_tok[order])
    row_gate = jnp.zeros((n_rows,), x.dtype).at[dest].set(gates.reshape(-1)[order])
    block_e = jnp.minimum(
        jnp.searchsorted(padded_end, jnp.arange(n_blocks, dtype=padded_end.dtype) * MOE_BLOCK, side='right'),
        N_EXPERTS - 1)
    x_rows = jnp.concatenate([xt, jnp.zeros((1, D), xt.dtype)], axis=0)[row_tok]
    x_rows = x_rows.reshape(n_blocks, MOE_BLOCK, D)

    def expert_block(args):
        xb, e = args
        gt = jnp.minimum(xb @ w_gate[e] + b_gate[e], SWIGLU_LIMIT)
        up = jnp.clip(xb @ w_up[e] + b_up[e], -SWIGLU_LIMIT, SWIGLU_LIMIT)
        hid = gt * jax.nn.sigmoid(SWIGLU_ALPHA * gt) * (up + 1.0)
        return hid @ w_down[e] + b_down[e]

    y_rows = lax.map(expert_block, (x_rows, block_e)).reshape(n_rows, D)
    out = jnp.zeros((T + 1, D), x.dtype).at[row_tok].add(y_rows * row_gate[:, None])[:T]
    return out.reshape(B, S, D)


def setup_inputs(seed: int = 0) -> dict:
    key = jax.random.key(seed)
    ks = jax.random.split(key, 24)
    f32 = jnp.float32

    def nrm(k, shape, scale):
        return jax.random.normal(k, (DEPTH,) + shape, f32) * scale

    x = jax.random.normal(ks[0], (BATCH, SEQ, D_MODEL), f32)
    col_scale = jnp.concatenate([
        jnp.ones((ATTN_WIDTH + KV_WIDTH,), f32),
        jnp.full((KV_WIDTH,), DEEPNORM_BETA, f32),
        jnp.full((GMLP_WIDTH,), DEEPNORM_BETA, f32),
        jnp.ones((GMLP_WIDTH,), f32)])
    w_in = nrm(ks[1], (D_MODEL, IN_WIDTH), D_MODEL ** -0.5) * col_scale
    b_in = nrm(ks[2], (IN_WIDTH,), 0.02)
    sinks = nrm(ks[3], (N_Q_HEADS,), 0.5)
    ln_v_g = 1.0 + nrm(ks[4], (GMLP_WIDTH,), 0.05)
    ln_v_b = nrm(ks[5], (GMLP_WIDTH,), 0.02)
    w_spatial = nrm(ks[6], (N_GMLP_GROUPS, CHUNK, CHUNK), CHUNK ** -0.5)
    b_spatial = 1.0 + nrm(ks[7], (N_GMLP_GROUPS, CHUNK), 0.05)
    w_out = nrm(ks[8], (MIX_WIDTH, D_MODEL), MIX_WIDTH ** -0.5 * DEEPNORM_BETA)
    b_out = nrm(ks[9], (D_MODEL,), 0.02)
    ln1_g = 1.0 + nrm(ks[10], (D_MODEL,), 0.05)
    ln1_b = nrm(ks[11], (D_MODEL,), 0.02)
    w_router = nrm(ks[12], (D_MODEL, N_EXPERTS), D_MODEL ** -0.5)
    b_router = nrm(ks[13], (N_EXPERTS,), 0.01)
    w_gate = nrm(ks[14], (N_EXPERTS, D_MODEL, D_FF), D_MODEL ** -0.5)
    b_gate = nrm(ks[15], (N_EXPERTS, D_FF), 0.02)
    w_up = nrm(ks[16], (N_EXPERTS, D_MODEL, D_FF), D_MODEL ** -0.5 * DEEPNORM_BETA)
    b_up = nrm(ks[17], (N_EXPERTS, D_FF), 0.02)
    w_down = nrm(ks[18], (N_EXPERTS, D_FF, D_MODEL), D_FF ** -0.5 * DEEPNORM_BETA)
    b_down = nrm(ks[19], (N_EXPERTS, D_MODEL), 0.02)
    ln2_g = 1.0 + nrm(ks[20], (D_MODEL,), 0.05)
    ln2_b = nrm(ks[21], (D_MODEL,), 0.02)
    return {"x": x, "w_in": w_in, "b_in": b_in, "sinks": sinks, "ln_v_g": ln_v_g, "ln_v_b": ln_v_b,
            "w_spatial": w_spatial, "b_spatial": b_spatial, "w_out": w_out, "b_out": b_out,
            "ln1_g": ln1_g, "ln1_b": ln1_b, "w_router": w_router, "b_router": b_router,
            "w_gate": w_gate, "b_gate": b_gate, "w_up": w_up, "b_up": b_up,
            "w_down": w_down, "b_down": b_down, "ln2_g": ln2_g, "ln2_b": ln2_b}


def reference(x, w_in, b_in, sinks, ln_v_g, ln_v_b, w_spatial, b_spatial, w_out, b_out,
              ln1_g, ln1_b, w_router, b_router, w_gate, b_gate, w_up, b_up,
              w_down, b_down, ln2_g, ln2_b):
    for l in range(DEPTH):
        mix = token_mixer(x, w_in[l], b_in[l], sinks[l], ln_v_g[l], ln_v_b[l],
                          w_spatial[l], b_spatial[l], w_out[l], b_out[l])
        h = layer_norm(DEEPNORM_ALPHA * x + mix, ln1_g[l], ln1_b[l])
        ffn = routed_experts(h, w_router[l], b_router[l], w_gate[l], b_gate[l],
                             w_up[l], b_up[l], w_down[l], b_down[l])
        x = layer_norm(DEEPNORM_ALPHA * h + ffn, ln2_g[l], ln2_b[l])
    return x
```

```python
import numpy as np
import concourse.bass as bass
import concourse.mybir as mybir
from concourse.bass_utils import run_bass_kernel_spmd

F32 = mybir.dt.float32
BF16 = mybir.dt.bfloat16
I32 = mybir.dt.int32
AF = mybir.ActivationFunctionType
ALU = mybir.AluOpType
AX = mybir.AxisListType

NCORES = 8
TOK = 2048
NB = 16
D = 1024
NE = 32
CAP = 384
CT = CAP // 128
ALPHA = 2.0 ** 0.25
EPS = 1e-5
NEG = -30000.0
GC0 = 0.044715 ** 0.5
GC1 = 1.5957691216057308
LAG_A = 150
KPRE = 4
LAG_C = 6


class Buf:
    def __init__(self, name, excl=False):
        self.name = name
        self.excl = excl
        self.w = None
        self.r = set()
        self.aw = set()


class Op:
    __slots__ = ("id", "eng", "fn", "deps", "dma", "dsi", "dval", "cost", "nbytes", "seq")


class Prog:
    ENGS = ["pe", "act", "dve", "pool", "sp"]
    ISSUE = {"sp": 0.06, "act": 0.06, "pool": 1.0, "pe": 0.06, "dve": 0.06}

    def __init__(self, nc, esems, dsems_hw, dsems_sw):
        self.nc = nc
        self.oplist = []
        self.esem = esems
        self.dsems = list(dsems_hw) + list(dsems_sw)
        self.nhw = len(dsems_hw)
        self.dcnt = [0] * len(self.dsems)
        self.dlast = [None] * len(self.dsems)
        self.dnext = {"hw": 0, "sw": 0}
        self.final = []

    @staticmethod
    def _cost(eng, c):
        if eng == "pe":
            return (128 if c is None else c) / 1900.0 + 0.02
        if eng == "act":
            return 0.22 + (512 if c is None else c) / 1400.0
        if eng == "dve":
            return 0.1 + (128 if c is None else c) / 960.0
        if eng == "pool":
            return 0.3 + 2.7 * (512 if c is None else c) / 1000.0
        return 0.05

    def op(self, eng, fn, reads=(), writes=(), accw=(), dma=False, c=None, nbytes=0):
        deps = set()
        for b in reads:
            if b.w is not None:
                deps.add(b.w)
            if not b.excl:
                deps |= b.aw
        for b in writes:
            if b.w is not None:
                deps.add(b.w)
            deps |= b.r
            deps |= b.aw
        for b in accw:
            if b.w is not None:
                deps.add(b.w)
            deps |= b.r
        o = Op()
        o.id = len(self.oplist)
        o.eng, o.fn, o.dma, o.nbytes, o.seq = eng, fn, dma, nbytes, 0
        o.cost = self._cost(eng, c)
        o.dsi = o.dval = None
        if dma:
            kind = "sw" if eng == "pool" else "hw"
            n = self.nhw if kind == "hw" else len(self.dsems) - self.nhw
            i = self.dnext[kind]
            self.dnext[kind] = (i + 1) % n
            if kind == "sw":
                i += self.nhw
            if self.dlast[i] is not None:
                deps.add(self.dlast[i])
            self.dcnt[i] += 16
            o.dsi, o.dval = i, self.dcnt[i]
            self.dlast[i] = o.id
        o.deps = deps
        self.oplist.append(o)
        for b in reads:
            if b.excl:
                b.w = o.id
            else:
                b.r.add(o.id)
        for b in writes:
            b.w = o.id
            b.r = set()
            b.aw = set()
        for b in accw:
            b.aw.add(o.id)
        return o.id

    def dma(self, eng, out, in_, reads=(), writes=(), accw=(), nbytes=65536, **kw):
        return self.op(eng, lambda e: e.dma_start(out=out, in_=in_, **kw), reads, writes, accw, dma=True,
                       nbytes=nbytes)

    def barrier_buf(self, b):
        return self.op("sp", lambda e: e.nop(), writes=[b], c=None)

    def barrier(self, bufs):
        o = Op()
        o.id = len(self.oplist)
        o.eng, o.fn, o.dma, o.nbytes, o.seq = "sp", (lambda e: e.nop()), False, 0, 0
        o.cost = 0.05
        o.dsi = o.dval = None
        o.deps = set(range(o.id))
        self.oplist.append(o)
        for b in bufs:
            b.w = o.id
        return o.id

    def schedule(self):
        import heapq
        ops = self.oplist
        n = len(ops)
        succ = [[] for _ in range(n)]
        nd = [0] * n
        for o in ops:
            nd[o.id] = len(o.deps)
            for d in o.deps:
                succ[d].append(o.id)
        bl = [0.0] * n
        for i in range(n - 1, -1, -1):
            m = 0.0
            for s_ in succ[i]:
                if bl[s_] > m:
                    m = bl[s_]
            o = ops[i]
            bl[i] = m + ((o.nbytes / 330e3 + 2.0) if o.dma else (o.cost + 0.12))
        ready = [0.0] * n
        future = {e: [] for e in self.ENGS}
        avail = {e: [] for e in self.ENGS}
        efree = {e: 0.0 for e in self.ENGS}
        order = {e: [] for e in self.ENGS}
        dma_free = 0.0
        for o in ops:
            if nd[o.id] == 0:
                heapq.heappush(future[o.eng], (0.0, -bl[o.id], o.id))
        done = 0
        while done < n:
            best = None
            for e in self.ENGS:
                t = efree[e]
                fu, av = future[e], avail[e]
                while fu and fu[0][0] <= t:
                    r_, k_, i_ = heapq.heappop(fu)
                    heapq.heappush(av, (k_, i_))
                if av:
                    cand = (t, av[0][0], e, True)
                elif fu:
                    cand = (fu[0][0], fu[0][1], e, False)
                else:
                    continue
                if best is None or cand[:2] < best[:2]:
                    best = cand
            start, k_, e, fa = best
            if fa:
                i = heapq.heappop(avail[e])[1]
            else:
                i = heapq.heappop(future[e])[2]
            o = ops[i]
            if o.dma:
                iss = self.ISSUE[e]
                efree[e] = start + iss
                s2 = max(start + iss, dma_free)
                dma_free = s2 + o.nbytes / 330e3
                fin = dma_free + 2.0
            else:
                efree[e] = start + o.cost
                fin = start + o.cost + 0.12
            order[e].append(i)
            done += 1
            for s in succ[i]:
                if fin > ready[s]:
                    ready[s] = fin
                nd[s] -= 1
                if nd[s] == 0:
                    heapq.heappush(future[ops[s].eng], (ready[s], -bl[s], s))
        self.sim_end = max(efree.values())
        return order

    def emit(self, block):
        order = self.schedule()
        ops = self.oplist
        for e in self.ENGS:
            k = 0
            for i in order[e]:
                if not ops[i].dma:
                    k += 1
                    ops[i].seq = k
        decos = {"pe": block.tensor, "act": block.scalar, "dve": block.vector,
                 "pool": block.gpsimd, "sp": block.sync}
        for ename in self.ENGS:
            plan = []
            seen = {}
            for i in order[ename]:
                o = ops[i]
                need = {}
                for di in o.deps:
                    d = ops[di]
                    if d.dma:
                        key, val = ("d", d.dsi), d.dval
                    else:
                        key, val = ("e", d.eng), d.seq
                        if d.eng == ename and not o.dma:
                            if ename == "pe":
                                continue
                    if need.get(key, 0) < val:
                        need[key] = val
                waits = []
                for key, val in need.items():
                    if seen.get(key, 0) >= val:
                        continue
                    seen[key] = val
                    sem = self.dsems[key[1]] if key[0] == "d" else self.esem[key[1]]
                    waits.append((sem, val))
                inc = (self.dsems[o.dsi], 16) if o.dma else (self.esem[ename], 1)
                plan.append((waits, o.fn, inc))
            fin = []
            if ename == "sp":
                for i in self.final:
                    fin.append((self.dsems[ops[i].dsi], ops[i].dval))

            def body(eh, plan=plan, fin=fin):
                for waits, fn, inc in plan:
                    for sem, val in waits:
                        eh.wait_ge(sem, val)
                    ins = fn(eh)
                    ins.then_inc(inc[0], inc[1])
                for sem, val in fin:
                    eh.wait_ge(sem, val)

            decos[ename](body)


class Arena:
    def __init__(self, t):
        self.t = t
        self.off = 0

    def reset(self):
        self.off = 0

    def f32(self, n):
        v = self.t[:, self.off:self.off + n]
        self.off += (n + 7) // 8 * 8
        assert self.off <= self.t.shape[1], (self.off, self.t.shape)
        return v

    def bf16(self, n):
        w = (n + 1) // 2
        v = self.t[:, self.off:self.off + w].bitcast(BF16)
        self.off += (w + 7) // 8 * 8
        assert self.off <= self.t.shape[1], (self.off, self.t.shape)
        return v


def build_nc():
    nc = bass.Bass("TRN2", target_bir_lowering=False)

    def din(name, shape):
        return nc.dram_tensor(name, list(shape), F32, kind="ExternalInput").ap()

    xT_d = din("xT", [D, TOK + 128])
    x_d = din("x", [TOK, D])
    mk_d = din("mk", [128, 384])
    cst_d = din("cst", [128, 512])
    cf_d = din("cf", [128, 32])
    w_in_d = din("w_in", [D, 1792])
    bqk_d = din("bqk", [128, 6])
    btok_d = din("btok", [1, 1152])
    snk_d = din("snk", [1, 8])
    lvg_d = din("lvg", [1, 512])
    lvb_d = din("lvb", [1, 512])
    wsT_d = din("wsT", [8, 128, 128])
    bsT_d = din("bsT", [128, 8])
    w_out_d = din("w_out", [D, D])
    bout_d = din("b_out", [1, D])
    l1g_d = din("l1g", [1, D])
    l1b_d = din("l1b", [1, D])
    l2g_d = din("l2g", [1, D])
    l2b_d = din("l2b", [1, D])
    wr_d = din("wr", [D, NE])
    br_d = din("br", [1, NE])
    wg_d = din("wg", [NE, D, D])
    wu_d = din("wu", [NE, D, D])
    wd_d = din("wd", [NE, D, D])
    bgT_d = din("bgT", [128, 256])
    buT_d = din("buT", [128, 256])
    bd_d = din("bd", [NE, D])
    y_d = nc.dram_tensor("y", [TOK, D], F32, kind="ExternalOutput").ap()
    cnt_d = nc.dram_tensor("cnt", [128, 32], F32, kind="ExternalOutput").ap()
    hbuf_d = nc.dram_tensor("hbuf", [TOK, D], F32, kind="Internal").ap()
    Xg_d = nc.dram_tensor("Xg", [NE * CAP, D], BF16, kind="Internal").ap()
    Y_d = nc.dram_tensor("Yd", [NE * CAP, D], F32, kind="Internal").ap()
    W16_d = nc.dram_tensor("W16", [KPRE * 3, D, D], BF16, kind="Internal").ap()

    from contextlib import ExitStack
    with ExitStack() as es:
        def sb(name, shape, dt):
            return es.enter_context(nc.sbuf_tensor("sb_" + name, list(shape), dt))

        arenaA_t = sb("arenaA", [128, 24576], F32)
        arenaB_t = sb("arenaB", [128, 18432], F32)
        cb = sb("cb", [128, 512], BF16)
        mkb = sb("mkb", [128, 384], BF16)
        eC = sb("eC", [128, 32], F32)
        bqk = sb("bqk", [128, 6], F32)
        esink = sb("esink", [128, 8], F32)
        bsT = sb("bsT", [128, 8], F32)
        btok = sb("btok", [1, 1152], BF16)
        boutb = sb("boutb", [1, D], BF16)
        brb = sb("brb", [1, NE], BF16)
        lvgB = sb("lvgB", [128, 512], F32)
        lvbB = sb("lvbB", [128, 512], F32)
        l1gB = sb("l1gB", [128, D], F32)
        l1bB = sb("l1bB", [128, D], F32)
        l2gB = sb("l2gB", [128, D], F32)
        l2bB = sb("l2bB", [128, D], F32)
        bgT = sb("bgT", [128, 256], F32)
        bu1 = sb("bu1", [128, 256], F32)
        idx_all = sb("idx_all", [128, NB * 4], I32)
        gate_all = sb("gate_all", [128, NB * 4], F32)
        base = sb("base", [128, 32], F32)
        neghalf = sb("neghalf", [128, 8], F32)
        ztile = sb("ztile", [128, D], BF16)
        bdrow = [sb("bdrow%d" % i, [1, D], BF16) for i in range(2)]
        sts = [sb("st%d" % i, [128, 128], F32) for i in range(2)]
        stC = [sb("stC%d" % i, [128, 128], F32) for i in range(3)]
        ps = es.enter_context(nc.psum_tensor("ps", [128, 8, 512], F32))
        esems = {e: es.enter_context(nc.semaphore("s_" + e)) for e in Prog.ENGS}
        dsems_hw = [es.enter_context(nc.semaphore("dh%d" % i)) for i in range(16)]
        dsems_sw = [es.enter_context(nc.semaphore("ds%d" % i)) for i in range(16)]
        block = es.enter_context(nc.Block())

        P = Prog(nc, esems, dsems_hw, dsems_sw)
        _breg = {}

        def breg(e):
            if "r" not in _breg:
                _breg["r"] = e.to_reg(NE * CAP - 1)
            return _breg["r"]

        A = Arena(arenaA_t)
        B = Arena(arenaB_t)
        bank = [Buf("bank%d" % i, excl=True) for i in range(8)]

        def psb(i):
            return ps[:, i, :]

        def psbf(i):
            return ps[:, i, :].bitcast(BF16)

        ident = cb[:, 0:128]
        lstrict = cb[:, 128:256]
        causT = cb[:, 256:384]
        ones_m = cb[:, 384:512]
        ones_row = cb[0:1, 384:512]
        xT_r = xT_d.rearrange("(k p) t -> p k t", p=128)

        def run_pipeline(gens, lag, maxflight):
            active = []
            nxt = 0
            while nxt < len(gens) or active:
                if nxt < len(gens) and len(active) < maxflight and (not active or active[-1][1] >= lag):
                    active.append([gens[nxt](), 0])
                    nxt += 1
                for item in list(active):
                    try:
                        next(item[0])
                        item[1] += 1
                    except StopIteration:
                        active.remove(item)

        b_cb, b_mk, b_small = Buf("cb"), Buf("mk"), Buf("small")
        P.dma("pool", cb[:], cst_d, writes=[b_cb])
        P.dma("pool", mkb[:], mk_d, writes=[b_mk])
        P.dma("pool", btok[:], btok_d, accw=[b_small])
        P.dma("pool", boutb[:], bout_d, accw=[b_small])
        P.dma("pool", brb[:], br_d, accw=[b_small])
        b_f = Buf("fconst")
        for dst, src in [(eC, cf_d), (bqk, bqk_d), (bsT, bsT_d), (bgT, bgT_d), (bu1, buT_d)]:
            P.dma("sp", dst[:], src, accw=[b_f])
        for dst, src in [(esink, snk_d), (lvgB, lvg_d), (lvbB, lvb_d), (l1gB, l1g_d), (l1bB, l1b_d),
                         (l2gB, l2g_d), (l2bB, l2b_d)]:
            P.dma("sp", dst[:], src.partition_broadcast(128)[:, 0, :], accw=[b_f])
        P.op("act", lambda e: e.activation(out=esink[:], in_=esink[:], func=AF.Exp), writes=[b_f])
        P.op("dve", lambda e: e.tensor_scalar(out=bu1[:], in0=bu1[:], scalar1=1.0, scalar2=None, op0=ALU.add),
             writes=[b_f])
        P.op("dve", lambda e: e.tensor_scalar(out=bsT[:], in0=bsT[:], scalar1=0.5, scalar2=None, op0=ALU.mult),
             writes=[b_f])
        P.op("dve", lambda e: e.memset(neghalf[:], -0.5), writes=[b_f])
        b_base = Buf("base")
        P.op("dve", lambda e: e.memset(base[:], 0.0), writes=[b_base])

        w_in_bf = A.bf16(8 * 1792).rearrange("p (k f) -> p k f", k=8)
        wkd = A.bf16(8 * 256).rearrange("p (k f) -> p k f", k=8)
        w_out_bf = A.bf16(8 * D).rearrange("p (k f) -> p k f", k=8)
        xTg = [A.bf16(8 * 512).rearrange("p (k t) -> p k t", k=8) for _ in range(2)]
        x_tok = [A.f32(D) for _ in range(2)]
        KT_f = B.bf16(2 * 2 * 3 * 128)
        KT = KT_f.rearrange("p (g j s) -> p g j s", g=2, j=2)
        Vaug_f = B.bf16(3 * 2 * 65)
        Vaug = Vaug_f.rearrange("p (s g d) -> p s g d", s=3, g=2)
        xTh = B.bf16(8 * 128).rearrange("p (k t) -> p k t", k=8)
        wsTm = B.bf16(8 * 128).rearrange("p (g t) -> p g t", g=8)
        wr_bf = B.bf16(8 * NE).rearrange("p (k e) -> p k e", k=8)
        TP = []
        for p in range(2):
            ar = A if p == 0 else B
            t = {}
            for n in ["tA", "tB", "tC", "tD", "ug", "gg", "gnf"]:
                t[n] = ar.f32(512)
            t["z"], t["hn"] = ar.f32(D), ar.f32(D)
            t["QT"], t["PT"] = B.bf16(512), B.bf16(2048)
            t["attn"], t["sgu"], t["gn"] = B.bf16(512), B.bf16(512), B.bf16(512)
            t["catT"], t["hT"] = B.bf16(1024), B.bf16(1024)
            t["h"] = B.f32(D)
            t["lg"], t["junk"], t["fl"], t["slotv"] = B.f32(32), B.f32(32), B.f32(32), B.f32(32)
            t["maskb"] = B.bf16(32)
            t["st"] = sts[p]
            t["b"] = {n: Buf(n + str(p)) for n in list(t.keys()) + ["xtok", "hn2", "tD2"]}
            t["xtok"] = x_tok[p]
            TP.append(t)

        hb4 = [B.bf16(D) for _ in range(4)]
        b_hb4 = [Buf("hb%d" % i) for i in range(4)]
        b_win, b_wkd, b_wout, b_ws, b_wr = Buf("w_in"), Buf("wkd"), Buf("w_out"), Buf("ws"), Buf("wr")
        w_in_r = w_in_d.rearrange("(k p) f -> p k f", p=128)
        w_out_r = w_out_d.rearrange("(k p) f -> p k f", p=128)
        for hf in range(2):
            P.dma("pool", w_in_bf[:, 4 * hf:4 * hf + 4, :], w_in_r[:, 4 * hf:4 * hf + 4, :], accw=[b_win])
        for g in range(2):
            for jj in range(2):
                P.op("dve", lambda e, g=g, jj=jj: e.tensor_copy(
                    out=wkd[:, :, g * 128 + jj * 64:g * 128 + jj * 64 + 64],
                    in_=w_in_bf[:, :, 512 + g * 64:512 + g * 64 + 64]), reads=[b_win], accw=[b_wkd])
        b_xth = Buf("xTh")
        for hf in range(2):
            P.dma("pool", xTh[:, 4 * hf:4 * hf + 4, :], xT_r[:, 4 * hf:4 * hf + 4, 0:128], accw=[b_xth])
        b_xtg = [Buf("xTg0"), Buf("xTg1")]

        def load_group(g):
            for hf in range(2):
                P.dma("pool", xTg[g % 2][:, 4 * hf:4 * hf + 4, :],
                      xT_r[:, 4 * hf:4 * hf + 4, 128 + g * 512:128 + (g + 1) * 512], accw=[b_xtg[g % 2]])

        load_group(0)
        for hf in range(2):
            P.dma("pool", w_out_bf[:, 4 * hf:4 * hf + 4, :], w_out_r[:, 4 * hf:4 * hf + 4, :],
                  reads=[b_win, b_xtg[0], b_xth], accw=[b_wout])
        wsT_r = wsT_d.rearrange("g s t -> s g t")
        for hf in range(2):
            P.dma("pool", wsTm[:, 4 * hf:4 * hf + 4, :], wsT_r[:, 4 * hf:4 * hf + 4, :],
                  reads=[b_win, b_xtg[0], b_xth], accw=[b_ws])
        for gi in range(8):
            P.op("dve", lambda e, gi=gi: e.scalar_tensor_tensor(out=wsTm[:, gi, :], in0=wsTm[:, gi, :], scalar=0.5,
                                                                in1=causT, op0=ALU.mult, op1=ALU.mult),
                 reads=[b_cb], writes=[b_ws])
        wr_r = wr_d.rearrange("(k p) e -> p k e", p=128)
        for hf in range(2):
            P.dma("pool", wr_bf[:, 4 * hf:4 * hf + 4, :], wr_r[:, 4 * hf:4 * hf + 4, :],
                  reads=[b_win, b_xtg[0], b_xth], accw=[b_wr])
        b_kt, b_v = [Buf("kt%d" % i) for i in range(3)], [Buf("v%d" % i) for i in range(3)]
        P.op("pool", lambda e: e.memset(KT_f, 0.0), writes=b_kt)
        P.op("pool", lambda e: e.memset(Vaug_f, 1.0), writes=b_v)

        b_hbuf = [Buf("hbuf%d" % i) for i in range(NB)]
        b_Xg = Buf("Xg")
        b_z = Buf("ztile")
        P.op("dve", lambda e: e.memset(ztile[:], 0.0), writes=[b_z])
        Xg_z = Xg_d.rearrange("(c p r) d -> c p r d", p=128, r=8)
        for ci in range(NE * CAP // 1024):
            P.dma("act", Xg_z[ci], ztile[:].unsqueeze(1).to_broadcast([128, 8, D]),
                  reads=[b_z, b_win, b_wout, b_xtg[0], b_xth], accw=[b_Xg], nbytes=2097152)
        zf_done = P.barrier_buf(b_Xg)
        b_pre = Buf("pre")
        b_pre.w = zf_done
        b_w16 = [Buf("w16_%d" % i) for i in range(KPRE * 3)]
        for e in range(KPRE):
            for mi, wd_ in enumerate((wg_d, wu_d, wd_d)):
                src_ = wd_[e].rearrange("(k p) f -> p k f", p=128)
                dst_ = W16_d[e * 3 + mi].rearrange("(k p) f -> p k f", p=128)
                for hf in range(2):
                    P.dma("pool", dst_[:, 4 * hf:4 * hf + 4, :], src_[:, 4 * hf:4 * hf + 4, :], reads=[b_pre],
                          accw=[b_w16[e * 3 + mi]], nbytes=3145728)
        b_Y = Buf("Y")
        b_idx, b_gate = Buf("idx"), Buf("gate")

        def gelu(srcbank, bk, t1, t2, dst, bt1, bt2, bdst, extra=()):
            P.op("act", lambda e: e.activation(out=t1, in_=srcbank, func=AF.Square, scale=GC0),
                 reads=[bk], writes=[bt1], c=512)
            yield
            P.op("dve", lambda e: e.scalar_tensor_tensor(out=t2, in0=t1, scalar=1.0, in1=srcbank,
                                                         op0=ALU.add, op1=ALU.mult),
                 reads=[bt1, bk], writes=[bt2] + list(extra), c=512)
            yield
            P.op("act", lambda e: e.activation(out=t1, in_=t2, func=AF.Tanh, scale=GC1 * 0.5),
                 reads=[bt2], writes=[bt1], c=512)
            yield
            P.op("dve", lambda e: e.scalar_tensor_tensor(out=dst, in0=t1, scalar=1.0, in1=srcbank,
                                                         op0=ALU.add, op1=ALU.mult),
                 reads=[bt1, bk], writes=[bdst], c=512)
            yield

        def ln_tail(mean_ap, var_ap, tmp_ap, rstd_ap, nb_ap, bst, n, eps):
            P.op("dve", lambda e: e.tensor_scalar(out=tmp_ap, in0=var_ap, scalar1=eps, scalar2=None, op0=ALU.add),
                 reads=[bst], writes=[bst], c=8)
            yield
            P.op("pool", lambda e: e.tensor_tensor(out=rstd_ap, in0=tmp_ap, in1=neghalf[:, 0:n], op=ALU.pow),
                 reads=[bst, b_f], writes=[bst], c=8)
            yield
            P.op("dve", lambda e: e.scalar_tensor_tensor(out=nb_ap, in0=mean_ap, scalar=-1.0, in1=rstd_ap,
                                                         op0=ALU.mult, op1=ALU.mult), reads=[bst], writes=[bst],
                 c=8)
            yield

        def affine(src, gB, bB, dst, F, b_lo, b_hi, bdst, split):
            for eng, lo, hi, bb, kk in (("dve", 0, split, b_lo, 2), ("pool", split, F, b_hi, 1)):
                P.op(eng, lambda e, lo=lo, hi=hi: e.tensor_tensor(out=src[:, lo:hi], in0=src[:, lo:hi],
                                                                  in1=gB[:, lo:hi], op=ALU.mult),
                     reads=[b_f], writes=[bb], c=kk * (hi - lo))
                yield
            for eng, lo, hi, bb, kk in (("dve", 0, split, b_lo, 2), ("pool", split, F, b_hi, 1)):
                P.op(eng, lambda e, lo=lo, hi=hi: e.tensor_tensor(out=dst[:, lo:hi], in0=src[:, lo:hi],
                                                                  in1=bB[:, lo:hi], op=ALU.add),
                     reads=[bb, b_f], accw=[bdst], c=kk * (hi - lo))
                yield

        def layer_norm(src, bsrc, gB, bB, dst, bdst, hn, bhn, st, bst, act_stats=False, split=640):
            if act_stats:
                P.op("act", lambda e: e.activation(out=hn, in_=src, func=AF.Copy, accum_out=st[:, 120:121]),
                     reads=[bsrc], writes=list(bhn) + [bst], c=1100)
                yield
                P.op("act", lambda e: e.activation(out=hn, in_=src, func=AF.Square, accum_out=st[:, 121:122]),
                     reads=[bsrc], writes=list(bhn) + [bst], c=1100)
                yield
                P.op("dve", lambda e: e.tensor_scalar(out=st[:, 120:121], in0=st[:, 120:121], scalar1=1.0 / D,
                                                      scalar2=None, op0=ALU.mult), reads=[bst], writes=[bst], c=8)
                yield
                P.op("dve", lambda e: e.tensor_tensor(out=st[:, 125:126], in0=st[:, 120:121], in1=st[:, 120:121],
                                                      op=ALU.mult), reads=[bst], writes=[bst], c=8)
                yield
                P.op("dve", lambda e: e.scalar_tensor_tensor(out=st[:, 121:122], in0=st[:, 121:122], scalar=1.0 / D,
                                                             in1=st[:, 125:126], op0=ALU.mult, op1=ALU.subtract),
                     reads=[bst], writes=[bst], c=8)
                yield
            else:
                for hf in range(2):
                    P.op("dve", lambda e, hf=hf: e.bn_stats(out=st[:, 108 + 6 * hf:114 + 6 * hf],
                                                            in_=src[:, hf * 512:(hf + 1) * 512]),
                         reads=[bsrc], writes=[bst], c=512)
                    yield
                P.op("dve", lambda e: e.bn_aggr(out=st[:, 120:122], in_=st[:, 108:120]), reads=[bst], writes=[bst],
                     c=16)
                yield
            yield from ln_tail(st[:, 120:121], st[:, 121:122], st[:, 122:123], st[:, 123:124], st[:, 124:125], bst, 1, EPS)
            P.op("act", lambda e: e.activation(out=hn, in_=src, func=AF.Identity, bias=st[:, 124:125],
                                               scale=st[:, 123:124]), reads=[bsrc, bst], writes=list(bhn), c=1024)
            yield
            yield from affine(hn, gB, bB, dst, D, bhn[0], bhn[1], bdst, split)

        def blockA(b):
            g = b // 4
            s_cur = (b + 1) % 3
            s_prev = b % 3
            p = b % 2 if b >= 0 else 1
            T = TP[p]
            tb = T["b"]
            st = T["st"]
            bst = tb["st"]
            a0, a1, a2, a3 = 4 * p, 4 * p + 1, 4 * p + 2, 4 * p + 3
            if b >= 0:
                xTb = lambda k: xTg[g % 2][:, k, (b % 4) * 128:(b % 4) * 128 + 128]
                bx = b_xtg[g % 2]
                if b % 4 == 0 and g + 1 < 4:
                    load_group(g + 1)
            else:
                xTb = lambda k: xTh[:, k, :]
                bx = b_xth
            for c in range(2):
                for k in range(8):
                    P.op("pe", lambda e, c=c, k=k: e.matmul(psb(a1)[:, c * 128:(c + 1) * 128],
                                                            lhsT=wkd[:, k, c * 128:(c + 1) * 128], rhs=xTb(k),
                                                            start=(k == 0), stop=(k == 7)),
                         reads=[b_wkd, bx, bank[a1]])
                    yield
            for k in range(8):
                P.op("pe", lambda e, k=k: e.matmul(psb(a1)[:, 256:384], lhsT=xTb(k), rhs=w_in_bf[:, k, 640:768],
                                                   start=(k == 0), stop=False), reads=[b_win, bx, bank[a1]])
                yield
            P.op("pe", lambda e: e.matmul(psb(a1)[:, 256:384], lhsT=ones_row, rhs=btok[0:1, 0:128],
                                          start=False, stop=True), reads=[b_cb, b_small, bank[a1]])
            yield
            for gq in range(2):
                for j in range(2):
                    P.op("act", lambda e, gq=gq, j=j: e.activation(
                        out=KT[64 * j:64 * j + 64, gq, j, s_cur * 128:(s_cur + 1) * 128],
                        in_=psb(a1)[64 * j:64 * j + 64, gq * 128:(gq + 1) * 128], func=AF.Identity,
                        bias=bqk[64 * j:64 * j + 64, 4 + gq:5 + gq], scale=1.0),
                        reads=[bank[a1], b_f], accw=[b_kt[s_cur]])
                    yield
            P.op("act", lambda e: e.activation(out=Vaug[:, s_cur, :, 0:64],
                                               in_=psb(a1)[:, 256:384].rearrange("p (g d) -> p g d", g=2),
                                               func=AF.Copy), reads=[bank[a1]], accw=[b_v[s_cur]])
            yield
            if b < 0:
                return
            xt = T["xtok"]
            P.dma("sp", xt, x_d[b * 128:(b + 1) * 128, :], writes=[tb["xtok"]])
            for c in range(4):
                for k in range(8):
                    P.op("pe", lambda e, c=c, k=k: e.matmul(psb(a0)[:, c * 128:(c + 1) * 128],
                                                            lhsT=w_in_bf[:, k, c * 128:(c + 1) * 128], rhs=xTb(k),
                                                            start=(k == 0), stop=(k == 7)),
                         reads=[b_win, bx, bank[a0]])
                    yield
            QT = T["QT"]
            for c in range(4):
                P.op("act", lambda e, c=c: e.activation(out=QT[:, c * 128:(c + 1) * 128],
                                                        in_=psb(a0)[:, c * 128:(c + 1) * 128], func=AF.Identity,
                                                        bias=bqk[:, c:c + 1], scale=1.0),
                     reads=[bank[a0], b_f], writes=[tb["QT"]] if c == 0 else (), accw=[tb["QT"]] if c else ())
                yield
            for bi, c0, o0 in [(a2, 768, 128), (a3, 1280, 640)]:
                for k in range(8):
                    P.op("pe", lambda e, bi=bi, c0=c0, k=k: e.matmul(psb(bi), lhsT=xTb(k),
                                                                     rhs=w_in_bf[:, k, c0:c0 + 512],
                                                                     start=(k == 0), stop=False),
                         reads=[b_win, bx, bank[bi]], c=512)
                    yield
                P.op("pe", lambda e, bi=bi, o0=o0: e.matmul(psb(bi), lhsT=ones_row, rhs=btok[0:1, o0:o0 + 512],
                                                            start=False, stop=True),
                     reads=[b_cb, b_small, bank[bi]], c=512)
                yield
            yield from gelu(psb(a2), bank[a2], T["tA"], T["tB"], T["ug"], tb["tA"], tb["tB"], tb["ug"])
            yield from gelu(psb(a3), bank[a3], T["tC"], T["tD"], T["gg"], tb["tC"], tb["tD"], tb["gg"], extra=[tb["tD2"]])
            PT = T["PT"]
            for j in range(2):
                for c in range(4):
                    for kb in range(2):
                        i = c * 2 + kb
                        bi = a0 + i // 4
                        col = (i % 4) * 128
                        sl = s_prev if kb == 0 else s_cur
                        m = (2 if b == 0 else 0) if kb == 0 else 1
                        P.op("pe", lambda e, bi=bi, col=col, c=c, j=j, sl=sl: e.matmul(
                            psb(bi)[:, col:col + 128], lhsT=KT[:, c // 2, j, sl * 128:(sl + 1) * 128],
                            rhs=QT[:, c * 128:(c + 1) * 128], start=True, stop=False),
                            reads=[b_kt[sl], tb["QT"], bank[bi]])
                        yield
                        P.op("pe", lambda e, bi=bi, col=col, m=m: e.matmul(
                            psb(bi)[:, col:col + 128], lhsT=ident, rhs=mkb[:, m * 128:(m + 1) * 128],
                            start=False, stop=True), reads=[b_cb, b_mk, bank[bi]])
                        yield
                P.op("act", lambda e, j=j: e.activation(
                    out=PT[:, j * 1024:(j + 1) * 1024].rearrange("p (a f) -> p a f", a=2),
                    in_=ps[:, a0:a0 + 2, :], func=AF.Exp, scale=0.125), c=1024,
                    reads=[bank[a0], bank[a1]],
                    writes=[tb["PT"]] if j == 0 else (), accw=[tb["PT"]] if j else ())
                yield
            gg, gnf, tC, tD = T["gg"], T["gnf"], T["tC"], T["tD"]
            stv = lambda a, n=8: st[:, a:a + n]
            gg3 = gg.rearrange("p (g d) -> p g d", g=8)
            P.op("dve", lambda e: e.tensor_reduce(out=stv(0), in_=gg3, axis=AX.X, op=ALU.add),
                 reads=[tb["gg"]], writes=[bst], c=512)
            yield
            P.op("act", lambda e: e.activation(out=tC, in_=gg, func=AF.Square), reads=[tb["gg"]],
                 writes=[tb["tC"]])
            yield
            P.op("dve", lambda e: e.tensor_reduce(out=stv(8), in_=tC.rearrange("p (g d) -> p g d", g=8),
                                                  axis=AX.X, op=ALU.add), reads=[tb["tC"]], writes=[bst], c=512)
            yield
            P.op("dve", lambda e: e.tensor_scalar(out=stv(16), in0=stv(0), scalar1=1.0 / 64, scalar2=None,
                                                  op0=ALU.mult), reads=[bst], writes=[bst])
            yield
            P.op("dve", lambda e: e.tensor_tensor(out=stv(24), in0=stv(16), in1=stv(16), op=ALU.mult),
                 reads=[bst], writes=[bst])
            yield
            P.op("dve", lambda e: e.scalar_tensor_tensor(out=stv(32), in0=stv(8), scalar=1.0 / 64, in1=stv(24),
                                                         op0=ALU.mult, op1=ALU.subtract), reads=[bst], writes=[bst])
            yield
            yield from ln_tail(stv(16), stv(32), stv(40), stv(48), stv(56), bst, 8, 4.0 * EPS)
            for gi in range(8):
                P.op("dve", lambda e, gi=gi: e.tensor_scalar(out=gnf[:, gi * 64:(gi + 1) * 64],
                                                             in0=gg[:, gi * 64:(gi + 1) * 64],
                                                             scalar1=st[:, 48 + gi:49 + gi],
                                                             scalar2=st[:, 56 + gi:57 + gi],
                                                             op0=ALU.mult, op1=ALU.add),
                     reads=[tb["gg"], bst], writes=[tb["gnf"]] if gi == 0 else (),
                     accw=[tb["gnf"]] if gi else (), c=64)
                yield
            gn_bf = T["gn"]
            for eng, lo, hi, bb, kk in (("dve", 0, 320, tb["tD"], 2), ("pool", 320, 512, tb["tD2"], 1)):
                P.op(eng, lambda e, lo=lo, hi=hi: e.tensor_tensor(out=tD[:, lo:hi], in0=gnf[:, lo:hi],
                                                                  in1=lvgB[:, lo:hi], op=ALU.mult),
                     reads=[tb["gnf"], b_f], writes=[bb], c=kk * (hi - lo))
                yield
            for eng, lo, hi, bb, kk in (("dve", 0, 320, tb["tD"], 2), ("pool", 320, 512, tb["tD2"], 1)):
                P.op(eng, lambda e, lo=lo, hi=hi: e.tensor_tensor(out=gn_bf[:, lo:hi], in0=tD[:, lo:hi],
                                                                  in1=lvbB[:, lo:hi], op=ALU.add),
                     reads=[bb, b_f], accw=[tb["gn"]], c=kk * (hi - lo))
                yield
            for h in range(8):
                c, j = h // 2, h % 2
                gq = c // 2
                bi = a0 + h // 4
                hh = h % 4
                for kb in range(2):
                    sl = s_prev if kb == 0 else s_cur
                    i = c * 2 + kb
                    P.op("pe", lambda e, bi=bi, hh=hh, j=j, i=i, sl=sl, gq=gq, kb=kb: e.matmul(
                        psb(bi)[:, hh * 65:(hh + 1) * 65], lhsT=PT[:, j * 1024 + i * 128:j * 1024 + (i + 1) * 128],
                        rhs=Vaug[:, sl, gq, :], start=(kb == 0), stop=(kb == 1)),
                        reads=[tb["PT"], b_v[sl], bank[bi]])
                    yield
            attn_bf = T["attn"]
            for hf in range(2):
                Ov = psb(a0 + hf)[:, 0:260].rearrange("p (h d) -> p h d", h=4)
                P.op("dve", lambda e, Ov=Ov, hf=hf: e.tensor_tensor(
                    out=st[:, 64 + 4 * hf:68 + 4 * hf].unsqueeze(2), in0=Ov[:, :, 64:65],
                    in1=esink[:, 4 * hf:4 * hf + 4].unsqueeze(2), op=ALU.add),
                    reads=[bank[a0 + hf], b_f], writes=[bst])
                yield
                P.op("dve", lambda e, hf=hf: e.reciprocal(out=st[:, 72 + 4 * hf:76 + 4 * hf],
                                                          in_=st[:, 64 + 4 * hf:68 + 4 * hf]),
                     reads=[bst], writes=[bst])
                yield
                P.op("dve", lambda e, Ov=Ov, hf=hf: e.tensor_tensor(
                    out=attn_bf[:, hf * 256:(hf + 1) * 256].rearrange("p (h d) -> p h d", h=4),
                    in0=Ov[:, :, 0:64],
                    in1=st[:, 72 + 4 * hf:76 + 4 * hf].unsqueeze(2).to_broadcast([128, 4, 64]), op=ALU.mult),
                    reads=[bank[a0 + hf], bst], writes=[tb["attn"]] if hf == 0 else (),
                    accw=[tb["attn"]] if hf else (), c=256)
                yield
            for gi in range(8):
                P.op("pe", lambda e, gi=gi: e.matmul(psb(a2)[:, gi * 64:(gi + 1) * 64], lhsT=wsTm[:, gi, :],
                                                     rhs=gn_bf[:, gi * 64:(gi + 1) * 64], start=True, stop=True),
                     reads=[b_ws, tb["gn"], bank[a2]], c=64)
                yield
            sgu_bf, ug = T["sgu"], T["ug"]
            for gi in range(8):
                P.op("dve", lambda e, gi=gi: e.scalar_tensor_tensor(
                    out=sgu_bf[:, gi * 64:(gi + 1) * 64], in0=psb(a2)[:, gi * 64:(gi + 1) * 64],
                    scalar=bsT[:, gi:gi + 1], in1=ug[:, gi * 64:(gi + 1) * 64], op0=ALU.add, op1=ALU.mult),
                    reads=[bank[a2], b_f, tb["ug"]], writes=[tb["sgu"]] if gi == 0 else (),
                    accw=[tb["sgu"]] if gi else (), c=64)
                yield
            catT = T["catT"]
            for i in range(8):
                src = attn_bf if i < 4 else sgu_bf
                bsrc = tb["attn"] if i < 4 else tb["sgu"]
                ii = i % 4
                P.op("pe", lambda e, i=i, ii=ii, src=src: e.transpose(
                    out=psbf(a3)[:, i * 128:(i + 1) * 128], in_=src[:, ii * 128:(ii + 1) * 128], identity=ident),
                    reads=[bsrc, b_cb, bank[a3]])
                yield
            P.op("act", lambda e: e.activation(out=catT, in_=psbf(a3), func=AF.Copy), reads=[bank[a3]],
                 writes=[tb["catT"]], c=1024)
            yield
            zb = T["z"]
            for hf in range(2):
                for fc in range(8):
                    P.op("pe", lambda e, hf=hf, fc=fc: e.matmul(psb(a0 + hf), lhsT=catT[:, fc * 128:(fc + 1) * 128],
                                                                rhs=w_out_bf[:, fc, hf * 512:(hf + 1) * 512],
                                                                start=(fc == 0), stop=False),
                         reads=[tb["catT"], b_wout, bank[a0 + hf]], c=512)
                    yield
                P.op("pe", lambda e, hf=hf: e.matmul(psb(a0 + hf), lhsT=ones_row,
                                                     rhs=boutb[0:1, hf * 512:(hf + 1) * 512], start=False, stop=True),
                     reads=[b_cb, b_small, bank[a0 + hf]], c=512)
                yield
                P.op("dve", lambda e, hf=hf: e.scalar_tensor_tensor(
                    out=zb[:, hf * 512:(hf + 1) * 512], in0=xt[:, hf * 512:(hf + 1) * 512], scalar=ALPHA,
                    in1=psb(a0 + hf), op0=ALU.mult, op1=ALU.add),
                    reads=[tb["xtok"], bank[a0 + hf]], writes=[tb["z"]] if hf == 0 else (),
                    accw=[tb["z"]] if hf else (), c=512)
                yield
            h, hb = T["h"], hb4[b % 4]
            bhb = b_hb4[b % 4]
            yield from layer_norm(zb, tb["z"], l1gB, l1bB, h, tb["h"], T["hn"], (tb["hn"], tb["hn2"]), st, bst)
            P.dma("sp", hbuf_d[b * 128:(b + 1) * 128, :], h, reads=[tb["h"]], writes=[b_hbuf[b]])
            P.op("act", lambda e: e.activation(out=hb, in_=h, func=AF.Copy), reads=[tb["h"]], writes=[bhb],
                 c=1024)
            yield
            hT = T["hT"]
            for k in range(8):
                P.op("pe", lambda e, k=k: e.transpose(out=psbf(a2)[:, k * 128:(k + 1) * 128],
                                                      in_=hb[:, k * 128:(k + 1) * 128], identity=ident),
                     reads=[bhb, b_cb, bank[a2]])
                yield
            P.op("act", lambda e: e.activation(out=hT, in_=psbf(a2), func=AF.Copy), reads=[bank[a2]],
                 writes=[tb["hT"]], c=1024)
            yield
            for k in range(8):
                P.op("pe", lambda e, k=k: e.matmul(psb(a3)[:, 0:32], lhsT=hT[:, k * 128:(k + 1) * 128],
                                                   rhs=wr_bf[:, k, :], start=(k == 0), stop=False),
                     reads=[tb["hT"], b_wr, bank[a3]])
                yield
            P.op("pe", lambda e: e.matmul(psb(a3)[:, 0:32], lhsT=ones_row, rhs=brb[0:1, :], start=False, stop=True),
                 reads=[b_cb, b_small, bank[a3]])
            yield
            lg, junk, fl, slotv, maskb = T["lg"], T["junk"], T["fl"], T["slotv"], T["maskb"]
            P.op("dve", lambda e: e.tensor_copy(out=lg, in_=psb(a3)[:, 0:32]), reads=[bank[a3]], writes=[tb["lg"]])
            yield
            P.op("dve", lambda e: e.max(out=stv(80), in_=lg), reads=[tb["lg"]], writes=[bst])
            yield
            P.op("dve", lambda e: e.tensor_scalar(out=maskb, in0=lg, scalar1=st[:, 83:84], scalar2=None,
                                                  op0=ALU.is_ge), reads=[tb["lg"], bst], writes=[tb["maskb"]])
            yield
            P.op("pe", lambda e: e.matmul(psb(a3)[:, 32:64], lhsT=lstrict, rhs=maskb, start=True, stop=True),
                 reads=[b_cb, tb["maskb"], bank[a3]])
            yield
            P.op("pe", lambda e: e.matmul(psb(a3)[:, 64:96], lhsT=ones_m, rhs=maskb, start=True, stop=True),
                 reads=[b_cb, tb["maskb"], bank[a3]])
            yield
            P.op("dve", lambda e: e.tensor_tensor(out=slotv, in0=psb(a3)[:, 32:64], in1=base[:], op=ALU.add),
                 reads=[bank[a3], b_base], writes=[tb["slotv"]])
            yield
            P.op("dve", lambda e: e.tensor_tensor(out=base[:], in0=psb(a3)[:, 64:96], in1=base[:], op=ALU.add),
                 reads=[bank[a3]], writes=[b_base])
            yield
            P.op("dve", lambda e: e.tensor_scalar(out=fl, in0=slotv, scalar1=float(CAP), scalar2=1.0e6,
                                                  op0=ALU.is_ge, op1=ALU.mult),
                 reads=[tb["slotv"]], writes=[tb["fl"]])
            yield
            P.op("dve", lambda e: e.tensor_tensor(out=fl, in0=fl, in1=slotv, op=ALU.add),
                 reads=[tb["slotv"]], writes=[tb["fl"]])
            yield
            P.op("dve", lambda e: e.tensor_tensor(out=fl, in0=fl, in1=eC[:], op=ALU.add),
                 reads=[b_f], writes=[tb["fl"]])
            yield
            for j in range(4):
                P.op("dve", lambda e, j=j: e.scalar_tensor_tensor(
                    out=junk, in0=lg, scalar=st[:, 80 + j:81 + j], in1=fl, op0=ALU.is_equal, op1=ALU.mult,
                    accum_out=st[:, 96 + j:97 + j]), reads=[tb["lg"], tb["fl"], bst],
                    writes=[tb["junk"], bst])
                yield
            P.op("dve", lambda e: e.tensor_copy(out=idx_all[:, b * 4:(b + 1) * 4], in_=st[:, 96:100]),
                 reads=[bst], accw=[b_idx])
            yield
            P.op("dve", lambda e: e.tensor_scalar(out=st[:, 100:101], in0=st[:, 80:81], scalar1=-1.0,
                                                  scalar2=None, op0=ALU.mult), reads=[bst], writes=[bst])
            yield
            P.op("act", lambda e: e.activation(out=st[:, 104:108], in_=st[:, 80:84], func=AF.Exp,
                                               bias=st[:, 100:101], scale=1.0, accum_out=st[:, 101:102]),
                 reads=[bst], writes=[bst])
            yield
            P.op("dve", lambda e: e.reciprocal(out=st[:, 102:103], in_=st[:, 101:102]), reads=[bst], writes=[bst])
            yield
            P.op("dve", lambda e: e.tensor_scalar(out=st[:, 104:108], in0=st[:, 104:108],
                                                  scalar1=st[:, 102:103], scalar2=None, op0=ALU.mult),
                 reads=[bst], writes=[bst])
            yield
            P.op("dve", lambda e: e.scalar_tensor_tensor(out=gate_all[:, b * 4:(b + 1) * 4], in0=st[:, 96:100],
                                                         scalar=float(NE * CAP), in1=st[:, 104:108],
                                                         op0=ALU.is_lt, op1=ALU.mult),
                 reads=[bst], accw=[b_gate])
            yield
            for j in range(4):
                P.op("pool", lambda e, j=j: e.indirect_dma_start(
                    out=Xg_d[:, :], out_offset=bass.IndirectOffsetOnAxis(ap=idx_all[:, b * 4 + j:b * 4 + j + 1], axis=0),
                    in_=hb, in_offset=None, bounds_check=breg(e), oob_is_err=False),
                    reads=[bhb, b_idx], accw=[b_Xg], dma=True, nbytes=262144)
                yield

        for _ in blockA(-1):
            pass
        run_pipeline([(lambda b=b: blockA(b)) for b in range(NB)], lag=0, maxflight=1)
        P.dma("sp", cnt_d, base[:], reads=[b_base])

        A.reset()
        B.reset()
        wring = [A.bf16(8 * D).rearrange("p (k f) -> p k f", k=8) for _ in range(6)]
        b_wring = [Buf("wring%d" % i) for i in range(6)]
        Xe = [B.bf16(CT * D).rearrange("p (r d) -> p r d", r=CT) for _ in range(2)]
        XeT = [B.bf16(8 * CAP).rearrange("p (k c) -> p k c", k=8) for _ in range(2)]
        hidT = [B.bf16(8 * CAP).rearrange("p (k c) -> p k c", k=8) for _ in range(2)]
        gts = [B.f32(CAP) for _ in range(2)]
        sgs = [B.f32(CAP) for _ in range(2)]
        u1s = [B.f32(CAP) for _ in range(2)]
        gss = [B.f32(CAP) for _ in range(2)]
        ysb = [B.f32(D) for _ in range(2)]
        b_Xe, b_XeT, b_hid = ([Buf(n + str(i)) for i in range(2)] for n in ("Xe", "XeT", "hidT"))
        b_gt, b_sg, b_u1, b_gs = ([Buf(n + str(i)) for i in range(2)] for n in ("gt", "sg", "u1", "gs"))
        b_ysb = [Buf("ysb0"), Buf("ysb1")]
        b_bdB = [Buf("bdB0"), Buf("bdB1")]
        P.barrier(b_wring[4:] + b_Xe + b_XeT + b_hid + b_gt + b_sg + b_u1 + b_gs + b_ysb + b_bdB)
        for slot, olds in ((0, [b_win]), (1, [b_win, b_wkd]), (2, [b_wout]), (3, b_xtg)):
            for ob in olds:
                b_wring[slot].r |= ob.r | ob.aw
                if ob.w is not None:
                    b_wring[slot].r.add(ob.w)

        def load_w(e):
            for mi, wd_ in enumerate((wg_d, wu_d, wd_d)):
                slot = (e % 2) * 3 + mi
                if e < KPRE:
                    wr_ = W16_d[e * 3 + mi].rearrange("(k p) f -> p k f", p=128)
                    rd_, nb_ = [b_w16[e * 3 + mi]], 1048576
                else:
                    wr_ = wd_[e].rearrange("(k p) f -> p k f", p=128)
                    rd_, nb_ = [], 2097152
                for hf in range(2):
                    P.dma("pool", wring[slot][:, 4 * hf:4 * hf + 4, :], wr_[:, 4 * hf:4 * hf + 4, :],
                          reads=rd_, accw=[b_wring[slot]], nbytes=nb_, single_packet=True)
            P.dma("pool", bdrow[e % 2][:], bd_d[e:e + 1, :], writes=[b_bdB[e % 2]], nbytes=4096)

        def front(e):
            q2 = e % 2
            P.dma("sp", Xe[q2][:], Xg_d[e * CAP:(e + 1) * CAP, :].rearrange("(r p) d -> p r d", p=128),
                  reads=[b_Xg], writes=[b_Xe[q2]], nbytes=CAP * 2048)

        def front_pe(e):
            q2 = e % 2
            for kp in range(4):
                bi = kp % 2
                for kk in range(2):
                    k = 2 * kp + kk
                    for r in range(CT):
                        P.op("pe", lambda e_, bi=bi, kk=kk, r=r, k=k: e_.transpose(
                            out=psbf(bi)[:, kk * CAP + r * 128:kk * CAP + (r + 1) * 128],
                            in_=Xe[q2][:, r, k * 128:(k + 1) * 128], identity=ident),
                            reads=[b_Xe[q2], b_cb, bank[bi]])
                P.op("act", lambda e_, bi=bi, kp=kp: e_.activation(
                    out=XeT[q2][:, 2 * kp:2 * kp + 2, :],
                    in_=psbf(bi)[:, 0:2 * CAP].rearrange("p (k c) -> p k c", k=2),
                    func=AF.Copy), reads=[bank[bi]], writes=[b_XeT[q2]] if kp == 0 else (),
                    accw=[b_XeT[q2]] if kp else (), c=2 * CAP)

        def gate_up(e):
            q2 = e % 2
            wg_s, wu_s = wring[q2 * 3], wring[q2 * 3 + 1]
            bwg, bwu = b_wring[q2 * 3], b_wring[q2 * 3 + 1]
            for fc in range(8):
                pb = 2 + 2 * (fc % 2)
                for wi, (w_s, bw) in enumerate(((wg_s, bwg), (wu_s, bwu))):
                    for k in range(8):
                        P.op("pe", lambda e_, pb=pb, wi=wi, w_s=w_s, k=k, fc=fc: e_.matmul(
                            psb(pb + wi)[:, 0:CAP], lhsT=w_s[:, k, fc * 128:(fc + 1) * 128], rhs=XeT[q2][:, k, :],
                            start=(k == 0), stop=(k == 7)), reads=[bw, b_XeT[q2], bank[pb + wi]], c=CAP)
                q = fc % 2
                col = e * 8 + fc
                P.op("dve", lambda e_, pb=pb, q=q, col=col: e_.tensor_scalar(
                    out=gts[q], in0=psb(pb)[:, 0:CAP], scalar1=bgT[:, col:col + 1], scalar2=7.0,
                    op0=ALU.add, op1=ALU.min), reads=[bank[pb], b_f], writes=[b_gt[q]], c=CAP)
                P.op("act", lambda e_, q=q: e_.activation(out=sgs[q], in_=gts[q], func=AF.Sigmoid, scale=1.702),
                     reads=[b_gt[q]], writes=[b_sg[q]], c=CAP)
                P.op("dve", lambda e_, pb=pb, q=q, col=col: e_.tensor_scalar(
                    out=u1s[q], in0=psb(pb + 1)[:, 0:CAP], scalar1=bu1[:, col:col + 1], scalar2=8.0,
                    op0=ALU.add, op1=ALU.min), reads=[bank[pb + 1], b_f], writes=[b_u1[q]], c=CAP)
                P.op("pool", lambda e_, q=q: e_.tensor_tensor(out=gss[q], in0=gts[q], in1=sgs[q], op=ALU.mult),
                     reads=[b_gt[q], b_sg[q]], writes=[b_gs[q]], c=CAP)
                P.op("dve", lambda e_, q=q, fc=fc: e_.scalar_tensor_tensor(
                    out=hidT[q2][:, fc, :], in0=u1s[q], scalar=-6.0, in1=gss[q], op0=ALU.max, op1=ALU.mult),
                    reads=[b_u1[q], b_gs[q]], writes=[b_hid[q2]] if fc == 0 else (),
                    accw=[b_hid[q2]] if fc else (), c=2 * CAP)

        def down(e):
            q2 = e % 2
            wd_s, bwd = wring[q2 * 3 + 2], b_wring[q2 * 3 + 2]
            for r in range(CT):
                yb = ysb[r % 2]
                for hf in range(2):
                    for fc in range(8):
                        P.op("pe", lambda e_, hf=hf, fc=fc, r=r: e_.matmul(
                            psb(6 + hf), lhsT=hidT[q2][:, fc, r * 128:(r + 1) * 128],
                            rhs=wd_s[:, fc, hf * 512:(hf + 1) * 512], start=(fc == 0), stop=False),
                            reads=[b_hid[q2], bwd, bank[6 + hf]], c=512)
                    P.op("pe", lambda e_, hf=hf: e_.matmul(
                        psb(6 + hf), lhsT=ones_row, rhs=bdrow[q2][0:1, hf * 512:(hf + 1) * 512],
                        start=False, stop=True), reads=[b_cb, b_bdB[q2], bank[6 + hf]], c=512)
                    P.op("act", lambda e_, hf=hf, yb=yb: e_.activation(
                        out=yb[:, hf * 512:(hf + 1) * 512], in_=psb(6 + hf), func=AF.Copy),
                        reads=[bank[6 + hf]], writes=[b_ysb[r % 2]] if hf == 0 else (),
                        accw=[b_ysb[r % 2]] if hf else (), c=512)
                P.dma("sp", Y_d[e * CAP + r * 128:e * CAP + (r + 1) * 128, :], yb, reads=[b_ysb[r % 2]],
                      accw=[b_Y], nbytes=524288)

        load_w(0)
        front(0)
        front_pe(0)
        for e in range(NE):
            if e + 1 < NE:
                load_w(e + 1)
                front(e + 1)
            gate_up(e)
            if e + 1 < NE:
                front_pe(e + 1)
            down(e)

        A.reset()
        NC3 = 3
        Yg = [[A.f32(D) for _ in range(4)] for _ in range(NC3)]
        hC = [A.f32(D) for _ in range(NC3)]
        acc = [A.f32(D) for _ in range(NC3)]
        yo = [A.f32(D) for _ in range(NC3)]
        hnC = [A.f32(D) for _ in range(NC3)]
        b_Yg = [[Buf("Yg%d%d" % (s, j)) for j in range(4)] for s in range(NC3)]
        b_hC = [Buf("hC%d" % i) for i in range(NC3)]
        b_acc = [Buf("acc%d" % i) for i in range(NC3)]
        b_yo = [Buf("yo%d" % i) for i in range(NC3)]
        b_hnC = [Buf("hnC%d" % i) for i in range(NC3)]
        b_hnC2 = [Buf("hnCb%d" % i) for i in range(NC3)]
        b_stC = [Buf("stC%d" % i) for i in range(NC3)]
        P.barrier([x for s in b_Yg for x in s] + b_hC + b_acc + b_yo + b_hnC + b_hnC2 + b_stC)

        for s_i in range(NC3):
            for j_i in range(4):
                P.op("dve" if j_i % 2 == 0 else "pool", lambda e, s_i=s_i, j_i=j_i: e.memset(Yg[s_i][j_i], 0.0),
                     writes=[b_Yg[s_i][j_i]], c=1024)

        def blockC(b):
            s = b % NC3
            P.dma("sp", hC[s], hbuf_d[b * 128:(b + 1) * 128, :], reads=[b_hbuf[b]], writes=[b_hC[s]], nbytes=524288)
            for j in range(4):
                P.op("pool", lambda e, j=j: e.indirect_dma_start(
                    out=Yg[s][j], out_offset=None, in_=Y_d[:, :],
                    in_offset=bass.IndirectOffsetOnAxis(ap=idx_all[:, b * 4 + j:b * 4 + j + 1], axis=0),
                    bounds_check=breg(e), oob_is_err=False),
                    reads=[b_Y, b_idx], writes=[b_Yg[s][j]], dma=True, nbytes=524288)
                yield
            P.op("act", lambda e: e.activation(out=acc[s], in_=hC[s], func=AF.Copy, scale=ALPHA),
                 reads=[b_hC[s]], writes=[b_acc[s]], c=1024)
            yield
            for j in range(4):
                P.op("dve", lambda e, j=j: e.scalar_tensor_tensor(
                    out=acc[s], in0=Yg[s][j], scalar=gate_all[:, b * 4 + j:b * 4 + j + 1], in1=acc[s],
                    op0=ALU.mult, op1=ALU.add), reads=[b_Yg[s][j], b_gate], writes=[b_acc[s]], c=1024)
                yield
            yield from layer_norm(acc[s], b_acc[s], l2gB, l2bB, yo[s], b_yo[s], hnC[s], (b_hnC[s], b_hnC2[s]), stC[s], b_stC[s],
                                  act_stats=True, split=512)
            tk = P.dma("sp", y_d[b * 128:(b + 1) * 128, :], yo[s], reads=[b_yo[s]], nbytes=524288)
            P.final.append(tk)
            yield

        run_pipeline([(lambda b=b: blockC(b)) for b in range(NB)], lag=0, maxflight=1)

        P.emit(block)
    return nc


_NC_CACHE = {}


def _prep_inputs(inp):
    f = lambda a: np.ascontiguousarray(np.asarray(a, dtype=np.float32))
    x = f(inp["x"])
    w_in = f(inp["w_in"])[0]
    b_in = f(inp["b_in"])[0]
    p = np.arange(128)
    bqk = np.zeros((128, 6), np.float32)
    for c in range(4):
        bqk[:, c] = b_in[c * 128 + p]
    for g in range(2):
        bqk[:, 4 + g] = b_in[512 + g * 64 + (p % 64)]
    s_i = np.arange(128)[:, None]
    t_i = np.arange(128)[None, :]
    mk_prev = np.where(s_i > t_i, 0.0, NEG).astype(np.float32)
    mk_cur = np.where(s_i <= t_i, 0.0, NEG).astype(np.float32)
    mk_none = np.full((128, 128), NEG, np.float32)
    cst = np.concatenate([np.eye(128, dtype=np.float32), (s_i < t_i).astype(np.float32),
                          (s_i <= t_i).astype(np.float32), np.ones((128, 128), np.float32)], axis=1)
    cf = np.tile((np.arange(NE, dtype=np.float32) * CAP)[None, :], (128, 1))
    shared = {
        "cst": cst, "cf": np.ascontiguousarray(cf),
        "w_in": w_in, "bqk": bqk, "btok": np.ascontiguousarray(b_in[None, 640:1792]),
        "snk": f(inp["sinks"]).reshape(1, 8),
        "lvg": f(inp["ln_v_g"]).reshape(1, 512), "lvb": f(inp["ln_v_b"]).reshape(1, 512),
        "wsT": np.ascontiguousarray(f(inp["w_spatial"])[0].transpose(0, 2, 1)),
        "bsT": np.ascontiguousarray(f(inp["b_spatial"])[0].T),
        "w_out": f(inp["w_out"])[0], "b_out": f(inp["b_out"]).reshape(1, D),
        "l1g": f(inp["ln1_g"]).reshape(1, D), "l1b": f(inp["ln1_b"]).reshape(1, D),
        "l2g": f(inp["ln2_g"]).reshape(1, D), "l2b": f(inp["ln2_b"]).reshape(1, D),
        "wr": f(inp["w_router"])[0], "br": f(inp["b_router"]).reshape(1, NE),
        "wg": f(inp["w_gate"])[0], "wu": f(inp["w_up"])[0], "wd": f(inp["w_down"])[0],
        "bgT": np.ascontiguousarray(f(inp["b_gate"])[0].reshape(NE, 8, 128).transpose(2, 0, 1).reshape(128, 256)),
        "buT": np.ascontiguousarray(f(inp["b_up"])[0].reshape(NE, 8, 128).transpose(2, 0, 1).reshape(128, 256)),
        "bd": f(inp["b_down"])[0],
    }
    maps = []
    for c in range(NCORES):
        bi, q = c // 4, c % 4
        t0 = q * TOK
        xs = x[bi, t0:t0 + TOK]
        halo = x[bi, t0 - 128:t0] if q > 0 else np.zeros((128, D), np.float32)
        xT = np.ascontiguousarray(np.concatenate([halo, xs], axis=0).T)
        mk = np.concatenate([mk_prev, mk_cur, mk_prev if q > 0 else mk_none], axis=1)
        m = dict(shared)
        m["xT"] = xT
        m["x"] = np.ascontiguousarray(xs)
        m["mk"] = np.ascontiguousarray(mk)
        maps.append(m)
    return maps


def kernel(**inputs):
    if "nc" not in _NC_CACHE:
        _NC_CACHE["nc"] = build_nc()
    nc = _NC_CACHE["nc"]
    maps = _prep_inputs(inputs)
    res = run_bass_kernel_spmd(nc, maps, core_ids=list(range(NCORES)))
    out = np.empty((2, 8192, D), np.float32)
    _NC_CACHE["cnt"] = [np.asarray(res.results[c]["cnt"])[0] for c in range(NCORES)]
    for c in range(NCORES):
        out[c // 4, (c % 4) * TOK:(c % 4 + 1) * TOK] = np.asarray(res.results[c]["y"], dtype=np.float32)
    return out
```

```python
import numpy as np
import concourse.bass as bass
import concourse.mybir as mybir
from concourse.bass_utils import run_bass_kernel_spmd

F32 = mybir.dt.float32
BF16 = mybir.dt.bfloat16
I32 = mybir.dt.int32
AF = mybir.ActivationFunctionType
ALU = mybir.AluOpType
AX = mybir.AxisListType

NCORES = 8
TOK = 2048
NB = 16
D = 1024
NE = 32
CAP = 384
CT = CAP // 128
ALPHA = 2.0 ** 0.25
EPS = 1e-5
NEG = -30000.0
GC0 = 0.044715 ** 0.5
GC1 = 1.5957691216057308
LAG_A = 150
KPRE = 4
LAG_C = 6


class Buf:
    def __init__(self, name, excl=False):
        self.name = name
        self.excl = excl
        self.w = None
        self.r = set()
        self.aw = set()


class Op:
    __slots__ = ("id", "eng", "fn", "deps", "dma", "dsi", "dval", "cost", "nbytes", "seq")


class Prog:
    ENGS = ["pe", "act", "dve", "pool", "sp"]
    ISSUE = {"sp": 0.06, "act": 0.06, "pool": 1.0, "pe": 0.06, "dve": 0.06}

    def __init__(self, nc, esems, dsems_hw, dsems_sw):
        self.nc = nc
        self.oplist = []
        self.esem = esems
        self.dsems = list(dsems_hw) + list(dsems_sw)
        self.nhw = len(dsems_hw)
        self.dcnt = [0] * len(self.dsems)
        self.dlast = [None] * len(self.dsems)
        self.dnext = {"hw": 0, "sw": 0}
        self.final = []

    @staticmethod
    def _cost(eng, c):
        if eng == "pe":
            return (128 if c is None else c) / 1900.0 + 0.02
        if eng == "act":
            return 0.22 + (512 if c is None else c) / 1400.0
        if eng == "dve":
            return 0.1 + (128 if c is None else c) / 960.0
        if eng == "pool":
            return 0.3 + 2.7 * (512 if c is None else c) / 1000.0
        return 0.05

    def op(self, eng, fn, reads=(), writes=(), accw=(), dma=False, c=None, nbytes=0):
        deps = set()
        for b in reads:
            if b.w is not None:
                deps.add(b.w)
            if not b.excl:
                deps |= b.aw
        for b in writes:
            if b.w is not None:
                deps.add(b.w)
            deps |= b.r
            deps |= b.aw
        for b in accw:
            if b.w is not None:
                deps.add(b.w)
            deps |= b.r
        o = Op()
        o.id = len(self.oplist)
        o.eng, o.fn, o.dma, o.nbytes, o.seq = eng, fn, dma, nbytes, 0
        o.cost = self._cost(eng, c)
        o.dsi = o.dval = None
        if dma:
            kind = "sw" if eng == "pool" else "hw"
            n = self.nhw if kind == "hw" else len(self.dsems) - self.nhw
            i = self.dnext[kind]
            self.dnext[kind] = (i + 1) % n
            if kind == "sw":
                i += self.nhw
            if self.dlast[i] is not None:
                deps.add(self.dlast[i])
            self.dcnt[i] += 16
            o.dsi, o.dval = i, self.dcnt[i]
            self.dlast[i] = o.id
        o.deps = deps
        self.oplist.append(o)
        for b in reads:
            if b.excl:
                b.w = o.id
            else:
                b.r.add(o.id)
        for b in writes:
            b.w = o.id
            b.r = set()
            b.aw = set()
        for b in accw:
            b.aw.add(o.id)
        return o.id

    def dma(self, eng, out, in_, reads=(), writes=(), accw=(), nbytes=65536, **kw):
        return self.op(eng, lambda e: e.dma_start(out=out, in_=in_, **kw), reads, writes, accw, dma=True,
                       nbytes=nbytes)

    def barrier_buf(self, b):
        self.op("sp", lambda e: e.nop(), writes=[b], c=None)

    def barrier(self, bufs):
        o = Op()
        o.id = len(self.oplist)
        o.eng, o.fn, o.dma, o.nbytes, o.seq = "sp", (lambda e: e.nop()), False, 0, 0
        o.cost = 0.05
        o.dsi = o.dval = None
        o.deps = set(range(o.id))
        self.oplist.append(o)
        for b in bufs:
            b.w = o.id
        return o.id

    def schedule(self):
        import heapq
        ops = self.oplist
        n = len(ops)
        succ = [[] for _ in range(n)]
        nd = [0] * n
        for o in ops:
            nd[o.id] = len(o.deps)
            for d in o.deps:
                succ[d].append(o.id)
        bl = [0.0] * n
        for i in range(n - 1, -1, -1):
            m = 0.0
            for s_ in succ[i]:
                if bl[s_] > m:
                    m = bl[s_]
            o = ops[i]
            bl[i] = m + ((o.nbytes / 330e3 + 2.0) if o.dma else (o.cost + 0.12))
        ready = [0.0] * n
        future = {e: [] for e in self.ENGS}
        avail = {e: [] for e in self.ENGS}
        efree = {e: 0.0 for e in self.ENGS}
        order = {e: [] for e in self.ENGS}
        dma_free = 0.0
        for o in ops:
            if nd[o.id] == 0:
                heapq.heappush(future[o.eng], (0.0, -bl[o.id], o.id))
        done = 0
        while done < n:
            best = None
            for e in self.ENGS:
                t = efree[e]
                fu, av = future[e], avail[e]
                while fu and fu[0][0] <= t:
                    r_, k_, i_ = heapq.heappop(fu)
                    heapq.heappush(av, (k_, i_))
                if av:
                    cand = (t, av[0][0], e, True)
                elif fu:
                    cand = (fu[0][0], fu[0][1], e, False)
                else:
                    continue
                if best is None or cand[:2] < best[:2]:
                    best = cand
            start, k_, e, fa = best
            if fa:
                i = heapq.heappop(avail[e])[1]
            else:
                i = heapq.heappop(future[e])[2]
            o = ops[i]
            if o.dma:
                iss = self.ISSUE[e]
                efree[e] = start + iss
                s2 = max(start + iss, dma_free)
                dma_free = s2 + o.nbytes / 330e3
                fin = dma_free + 2.0
            else:
                efree[e] = start + o.cost
                fin = start + o.cost + 0.12
            order[e].append(i)
            done += 1
            for s in succ[i]:
                if fin > ready[s]:
                    ready[s] = fin
                nd[s] -= 1
                if nd[s] == 0:
                    heapq.heappush(future[ops[s].eng], (ready[s], -bl[s], s))
        self.sim_end = max(efree.values())
        return order

    def emit(self, block):
        order = self.schedule()
        ops = self.oplist
        for e in self.ENGS:
            k = 0
            for i in order[e]:
                if not ops[i].dma:
                    k += 1
                    ops[i].seq = k
        decos = {"pe": block.tensor, "act": block.scalar, "dve": block.vector,
                 "pool": block.gpsimd, "sp": block.sync}
        for ename in self.ENGS:
            plan = []
            seen = {}
            for i in order[ename]:
                o = ops[i]
                need = {}
                for di in o.deps:
                    d = ops[di]
                    if d.dma:
                        key, val = ("d", d.dsi), d.dval
                    else:
                        key, val = ("e", d.eng), d.seq
                        if d.eng == ename and not o.dma:
                            if ename == "pe":
                                continue
                    if need.get(key, 0) < val:
                        need[key] = val
                waits = []
                for key, val in need.items():
                    if seen.get(key, 0) >= val:
                        continue
                    seen[key] = val
                    sem = self.dsems[key[1]] if key[0] == "d" else self.esem[key[1]]
                    waits.append((sem, val))
                inc = (self.dsems[o.dsi], 16) if o.dma else (self.esem[ename], 1)
                plan.append((waits, o.fn, inc))
            fin = []
            if ename == "sp":
                for i in self.final:
                    fin.append((self.dsems[ops[i].dsi], ops[i].dval))

            def body(eh, plan=plan, fin=fin):
                for waits, fn, inc in plan:
                    for sem, val in waits:
                        eh.wait_ge(sem, val)
                    ins = fn(eh)
                    ins.then_inc(inc[0], inc[1])
                for sem, val in fin:
                    eh.wait_ge(sem, val)

            decos[ename](body)


class Arena:
    def __init__(self, t):
        self.t = t
        self.off = 0

    def reset(self):
        self.off = 0

    def f32(self, n):
        v = self.t[:, self.off:self.off + n]
        self.off += (n + 7) // 8 * 8
        assert self.off <= self.t.shape[1], (self.off, self.t.shape)
        return v

    def bf16(self, n):
        w = (n + 1) // 2
        v = self.t[:, self.off:self.off + w].bitcast(BF16)
        self.off += (w + 7) // 8 * 8
        assert self.off <= self.t.shape[1], (self.off, self.t.shape)
        return v


def build_nc():
    nc = bass.Bass("TRN2", target_bir_lowering=False)

    def din(name, shape):
        return nc.dram_tensor(name, list(shape), F32, kind="ExternalInput").ap()

    xT_d = din("xT", [D, TOK + 128])
    x_d = din("x", [TOK, D])
    mk_d = din("mk", [128, 384])
    cst_d = din("cst", [128, 512])
    cf_d = din("cf", [128, 32])
    w_in_d = din("w_in", [D, 1792])
    bqk_d = din("bqk", [128, 6])
    btok_d = din("btok", [1, 1152])
    snk_d = din("snk", [1, 8])
    lvg_d = din("lvg", [1, 512])
    lvb_d = din("lvb", [1, 512])
    wsT_d = din("wsT", [8, 128, 128])
    bsT_d = din("bsT", [128, 8])
    w_out_d = din("w_out", [D, D])
    bout_d = din("b_out", [1, D])
    l1g_d = din("l1g", [1, D])
    l1b_d = din("l1b", [1, D])
    l2g_d = din("l2g", [1, D])
    l2b_d = din("l2b", [1, D])
    wr_d = din("wr", [D, NE])
    br_d = din("br", [1, NE])
    wg_d = din("wg", [NE, D, D])
    wu_d = din("wu", [NE, D, D])
    wd_d = din("wd", [NE, D, D])
    bgT_d = din("bgT", [128, 256])
    buT_d = din("buT", [128, 256])
    bd_d = din("bd", [NE, D])
    y_d = nc.dram_tensor("y", [TOK, D], F32, kind="ExternalOutput").ap()
    cnt_d = nc.dram_tensor("cnt", [128, 32], F32, kind="ExternalOutput").ap()
    hbuf_d = nc.dram_tensor("hbuf", [TOK, D], F32, kind="Internal").ap()
    Xg_d = nc.dram_tensor("Xg", [NE * CAP, D], BF16, kind="Internal").ap()
    Y_d = nc.dram_tensor("Yd", [NE * CAP, D], F32, kind="Internal").ap()
    W16_d = nc.dram_tensor("W16", [KPRE * 3, D, D], BF16, kind="Internal").ap()

    from contextlib import ExitStack
    with ExitStack() as es:
        def sb(name, shape, dt):
            return es.enter_context(nc.sbuf_tensor("sb_" + name, list(shape), dt))

        arenaA_t = sb("arenaA", [128, 24576], F32)
        arenaB_t = sb("arenaB", [128, 18432], F32)
        cb = sb("cb", [128, 512], BF16)
        mkb = sb("mkb", [128, 384], BF16)
        eC = sb("eC", [128, 32], F32)
        bqk = sb("bqk", [128, 6], F32)
        esink = sb("esink", [128, 8], F32)
        bsT = sb("bsT", [128, 8], F32)
        btok = sb("btok", [1, 1152], BF16)
        boutb = sb("boutb", [1, D], BF16)
        brb = sb("brb", [1, NE], BF16)
        lvgB = sb("lvgB", [128, 512], F32)
        lvbB = sb("lvbB", [128, 512], F32)
        l1gB = sb("l1gB", [128, D], F32)
        l1bB = sb("l1bB", [128, D], F32)
        l2gB = sb("l2gB", [128, D], F32)
        l2bB = sb("l2bB", [128, D], F32)
        bgT = sb("bgT", [128, 256], F32)
        bu1 = sb("bu1", [128, 256], F32)
        idx_all = sb("idx_all", [128, NB * 4], I32)
        gate_all = sb("gate_all", [128, NB * 4], F32)
        base = sb("base", [128, 32], F32)
        neghalf = sb("neghalf", [128, 8], F32)
        ztile = sb("ztile", [128, D], BF16)
        bdrow = [sb("bdrow%d" % i, [1, D], BF16) for i in range(2)]
        sts = [sb("st%d" % i, [128, 128], F32) for i in range(2)]
        stC = [sb("stC%d" % i, [128, 128], F32) for i in range(3)]
        ps = es.enter_context(nc.psum_tensor("ps", [128, 8, 512], F32))
        esems = {e: es.enter_context(nc.semaphore("s_" + e)) for e in Prog.ENGS}
        dsems_hw = [es.enter_context(nc.semaphore("dh%d" % i)) for i in range(16)]
        dsems_sw = [es.enter_context(nc.semaphore("ds%d" % i)) for i in range(16)]
        block = es.enter_context(nc.Block())

        P = Prog(nc, esems, dsems_hw, dsems_sw)
        _breg = {}

        def breg(e):
            if "r" not in _breg:
                _breg["r"] = e.to_reg(NE * CAP - 1)
            return _breg["r"]

        A = Arena(arenaA_t)
        B = Arena(arenaB_t)
        bank = [Buf("bank%d" % i, excl=True) for i in range(8)]

        def psb(i):
            return ps[:, i, :]

        def psbf(i):
            return ps[:, i, :].bitcast(BF16)

        ident = cb[:, 0:128]
        lstrict = cb[:, 128:256]
        causT = cb[:, 256:384]
        ones_m = cb[:, 384:512]
        ones_row = cb[0:1, 384:512]
        xT_r = xT_d.rearrange("(k p) t -> p k t", p=128)

        def run_pipeline(gens, lag, maxflight):
            active = []
            nxt = 0
            while nxt < len(gens) or active:
                if nxt < len(gens) and len(active) < maxflight and (not active or active[-1][1] >= lag):
                    active.append([gens[nxt](), 0])
                    nxt += 1
                for item in list(active):
                    try:
                        next(item[0])
                        item[1] += 1
                    except StopIteration:
                        active.remove(item)

        b_cb, b_mk, b_small = Buf("cb"), Buf("mk"), Buf("small")
        P.dma("pool", cb[:], cst_d, writes=[b_cb])
        P.dma("pool", mkb[:], mk_d, writes=[b_mk])
        P.dma("pool", btok[:], btok_d, accw=[b_small])
        P.dma("pool", boutb[:], bout_d, accw=[b_small])
        P.dma("pool", brb[:], br_d, accw=[b_small])
        b_f = Buf("fconst")
        for dst, src in [(eC, cf_d), (bqk, bqk_d), (bsT, bsT_d), (bgT, bgT_d), (bu1, buT_d)]:
            P.dma("sp", dst[:], src, accw=[b_f])
        for dst, src in [(esink, snk_d), (lvgB, lvg_d), (lvbB, lvb_d), (l1gB, l1g_d), (l1bB, l1b_d),
                         (l2gB, l2g_d), (l2bB, l2b_d)]:
            P.dma("sp", dst[:], src.partition_broadcast(128)[:, 0, :], accw=[b_f])
        P.op("act", lambda e: e.activation(out=esink[:], in_=esink[:], func=AF.Exp), writes=[b_f])
        P.op("dve", lambda e: e.tensor_scalar(out=bu1[:], in0=bu1[:], scalar1=1.0, scalar2=None, op0=ALU.add),
             writes=[b_f])
        P.op("dve", lambda e: e.tensor_scalar(out=bsT[:], in0=bsT[:], scalar1=0.5, scalar2=None, op0=ALU.mult),
             writes=[b_f])
        P.op("dve", lambda e: e.memset(neghalf[:], -0.5), writes=[b_f])
        b_base = Buf("base")
        P.op("dve", lambda e: e.memset(base[:], 0.0), writes=[b_base])

        w_in_bf = A.bf16(8 * 1792).rearrange("p (k f) -> p k f", k=8)
        wkd = A.bf16(8 * 256).rearrange("p (k f) -> p k f", k=8)
        w_out_bf = A.bf16(8 * D).rearrange("p (k f) -> p k f", k=8)
        xTg = [A.bf16(8 * 512).rearrange("p (k t) -> p k t", k=8) for _ in range(2)]
        x_tok = [A.f32(D) for _ in range(2)]
        KT_f = B.bf16(2 * 2 * 3 * 128)
        KT = KT_f.rearrange("p (g j s) -> p g j s", g=2, j=2)
        Vaug_f = B.bf16(3 * 2 * 65)
        Vaug = Vaug_f.rearrange("p (s g d) -> p s g d", s=3, g=2)
        xTh = B.bf16(8 * 128).rearrange("p (k t) -> p k t", k=8)
        wsTm = B.bf16(8 * 128).rearrange("p (g t) -> p g t", g=8)
        wr_bf = B.bf16(8 * NE).rearrange("p (k e) -> p k e", k=8)
        TP = []
        for p in range(2):
            ar = A if p == 0 else B
            t = {}
            for n in ["tA", "tB", "tC", "tD", "ug", "gg", "gnf"]:
                t[n] = ar.f32(512)
            t["z"], t["hn"] = ar.f32(D), ar.f32(D)
            t["QT"], t["PT"] = B.bf16(512), B.bf16(2048)
            t["attn"], t["sgu"], t["gn"] = B.bf16(512), B.bf16(512), B.bf16(512)
            t["catT"], t["hT"] = B.bf16(1024), B.bf16(1024)
            t["h"] = B.f32(D)
            t["lg"], t["junk"], t["fl"], t["slotv"] = B.f32(32), B.f32(32), B.f32(32), B.f32(32)
            t["maskb"] = B.bf16(32)
            t["st"] = sts[p]
            t["b"] = {n: Buf(n + str(p)) for n in list(t.keys()) + ["xtok", "hn2", "tD2"]}
            t["xtok"] = x_tok[p]
            TP.append(t)

        hb4 = [B.bf16(D) for _ in range(4)]
        b_hb4 = [Buf("hb%d" % i) for i in range(4)]
        b_win, b_wkd, b_wout, b_ws, b_wr = Buf("w_in"), Buf("wkd"), Buf("w_out"), Buf("ws"), Buf("wr")
        w_in_r = w_in_d.rearrange("(k p) f -> p k f", p=128)
        w_out_r = w_out_d.rearrange("(k p) f -> p k f", p=128)
        for hf in range(2):
            P.dma("pool", w_in_bf[:, 4 * hf:4 * hf + 4, :], w_in_r[:, 4 * hf:4 * hf + 4, :], accw=[b_win])
        for g in range(2):
            for jj in range(2):
                P.op("dve", lambda e, g=g, jj=jj: e.tensor_copy(
                    out=wkd[:, :, g * 128 + jj * 64:g * 128 + jj * 64 + 64],
                    in_=w_in_bf[:, :, 512 + g * 64:512 + g * 64 + 64]), reads=[b_win], accw=[b_wkd])
        b_xth = Buf("xTh")
        for hf in range(2):
            P.dma("pool", xTh[:, 4 * hf:4 * hf + 4, :], xT_r[:, 4 * hf:4 * hf + 4, 0:128], accw=[b_xth])
        b_xtg = [Buf("xTg0"), Buf("xTg1")]

        def load_group(g):
            for hf in range(2):
                P.dma("pool", xTg[g % 2][:, 4 * hf:4 * hf + 4, :],
                      xT_r[:, 4 * hf:4 * hf + 4, 128 + g * 512:128 + (g + 1) * 512], accw=[b_xtg[g % 2]])

        load_group(0)
        for hf in range(2):
            P.dma("pool", w_out_bf[:, 4 * hf:4 * hf + 4, :], w_out_r[:, 4 * hf:4 * hf + 4, :],
                  reads=[b_win, b_xtg[0], b_xth], accw=[b_wout])
        wsT_r = wsT_d.rearrange("g s t -> s g t")
        for hf in range(2):
            P.dma("pool", wsTm[:, 4 * hf:4 * hf + 4, :], wsT_r[:, 4 * hf:4 * hf + 4, :],
                  reads=[b_win, b_xtg[0], b_xth], accw=[b_ws])
        for gi in range(8):
            P.op("dve", lambda e, gi=gi: e.scalar_tensor_tensor(out=wsTm[:, gi, :], in0=wsTm[:, gi, :], scalar=0.5,
                                                                in1=causT, op0=ALU.mult, op1=ALU.mult),
                 reads=[b_cb], writes=[b_ws])
        wr_r = wr_d.rearrange("(k p) e -> p k e", p=128)
        for hf in range(2):
            P.dma("pool", wr_bf[:, 4 * hf:4 * hf + 4, :], wr_r[:, 4 * hf:4 * hf + 4, :],
                  reads=[b_win, b_xtg[0], b_xth], accw=[b_wr])
        b_kt, b_v = [Buf("kt%d" % i) for i in range(3)], [Buf("v%d" % i) for i in range(3)]
        P.op("pool", lambda e: e.memset(KT_f, 0.0), writes=b_kt)
        P.op("pool", lambda e: e.memset(Vaug_f, 1.0), writes=b_v)

        b_hbuf = [Buf("hbuf%d" % i) for i in range(NB)]
        b_Xg = Buf("Xg")
        b_z = Buf("ztile")
        P.op("dve", lambda e: e.memset(ztile[:], 0.0), writes=[b_z])
        Xg_z = Xg_d.rearrange("(c p r) d -> c p r d", p=128, r=8)
        for ci in range(NE * CAP // 1024):
            P.dma("act", Xg_z[ci], ztile[:].unsqueeze(1).to_broadcast([128, 8, D]),
                  reads=[b_z, b_win, b_wout, b_xtg[0], b_xth], accw=[b_Xg], nbytes=2097152)
        P.barrier_buf(b_Xg)
        b_Y = Buf("Y")
        b_idx, b_gate = Buf("idx"), Buf("gate")

        def gelu(srcbank, bk, t1, t2, dst, bt1, bt2, bdst, extra=()):
            P.op("act", lambda e: e.activation(out=t1, in_=srcbank, func=AF.Square, scale=GC0),
                 reads=[bk], writes=[bt1], c=512)
            yield
            P.op("dve", lambda e: e.scalar_tensor_tensor(out=t2, in0=t1, scalar=1.0, in1=srcbank,
                                                         op0=ALU.add, op1=ALU.mult),
                 reads=[bt1, bk], writes=[bt2] + list(extra), c=512)
            yield
            P.op("act", lambda e: e.activation(out=t1, in_=t2, func=AF.Tanh, scale=GC1 * 0.5),
                 reads=[bt2], writes=[bt1], c=512)
            yield
            P.op("dve", lambda e: e.scalar_tensor_tensor(out=dst, in0=t1, scalar=1.0, in1=srcbank,
                                                         op0=ALU.add, op1=ALU.mult),
                 reads=[bt1, bk], writes=[bdst], c=512)
            yield

        def ln_tail(mean_ap, var_ap, tmp_ap, rstd_ap, nb_ap, bst, n, eps):
            P.op("dve", lambda e: e.tensor_scalar(out=tmp_ap, in0=var_ap, scalar1=eps, scalar2=None, op0=ALU.add),
                 reads=[bst], writes=[bst], c=8)
            yield
            P.op("pool", lambda e: e.tensor_tensor(out=rstd_ap, in0=tmp_ap, in1=neghalf[:, 0:n], op=ALU.pow),
                 reads=[bst, b_f], writes=[bst], c=8)
            yield
            P.op("dve", lambda e: e.scalar_tensor_tensor(out=nb_ap, in0=mean_ap, scalar=-1.0, in1=rstd_ap,
                                                         op0=ALU.mult, op1=ALU.mult), reads=[bst], writes=[bst],
                 c=8)
            yield

        def affine(src, gB, bB, dst, F, b_lo, b_hi, bdst, split):
            for eng, lo, hi, bb, kk in (("dve", 0, split, b_lo, 2), ("pool", split, F, b_hi, 1)):
                P.op(eng, lambda e, lo=lo, hi=hi: e.tensor_tensor(out=src[:, lo:hi], in0=src[:, lo:hi],
                                                                  in1=gB[:, lo:hi], op=ALU.mult),
                     reads=[b_f], writes=[bb], c=kk * (hi - lo))
                yield
            for eng, lo, hi, bb, kk in (("dve", 0, split, b_lo, 2), ("pool", split, F, b_hi, 1)):
                P.op(eng, lambda e, lo=lo, hi=hi: e.tensor_tensor(out=dst[:, lo:hi], in0=src[:, lo:hi],
                                                                  in1=bB[:, lo:hi], op=ALU.add),
                     reads=[bb, b_f], accw=[bdst], c=kk * (hi - lo))
                yield

        def layer_norm(src, bsrc, gB, bB, dst, bdst, hn, bhn, st, bst, act_stats=False, split=640):
            if act_stats:
                P.op("act", lambda e: e.activation(out=hn, in_=src, func=AF.Copy, accum_out=st[:, 120:121]),
                     reads=[bsrc], writes=list(bhn) + [bst], c=1100)
                yield
                P.op("act", lambda e: e.activation(out=hn, in_=src, func=AF.Square, accum_out=st[:, 121:122]),
                     reads=[bsrc], writes=list(bhn) + [bst], c=1100)
                yield
                P.op("dve", lambda e: e.tensor_scalar(out=st[:, 120:121], in0=st[:, 120:121], scalar1=1.0 / D,
                                                      scalar2=None, op0=ALU.mult), reads=[bst], writes=[bst], c=8)
                yield
                P.op("dve", lambda e: e.tensor_tensor(out=st[:, 125:126], in0=st[:, 120:121], in1=st[:, 120:121],
                                                      op=ALU.mult), reads=[bst], writes=[bst], c=8)
                yield
                P.op("dve", lambda e: e.scalar_tensor_tensor(out=st[:, 121:122], in0=st[:, 121:122], scalar=1.0 / D,
                                                             in1=st[:, 125:126], op0=ALU.mult, op1=ALU.subtract),
                     reads=[bst], writes=[bst], c=8)
                yield
            else:
                for hf in range(2):
                    P.op("dve", lambda e, hf=hf: e.bn_stats(out=st[:, 108 + 6 * hf:114 + 6 * hf],
                                                            in_=src[:, hf * 512:(hf + 1) * 512]),
                         reads=[bsrc], writes=[bst], c=512)
                    yield
                P.op("dve", lambda e: e.bn_aggr(out=st[:, 120:122], in_=st[:, 108:120]), reads=[bst], writes=[bst],
                     c=16)
                yield
            yield from ln_tail(st[:, 120:121], st[:, 121:122], st[:, 122:123], st[:, 123:124], st[:, 124:125], bst, 1, EPS)
            P.op("act", lambda e: e.activation(out=hn, in_=src, func=AF.Identity, bias=st[:, 124:125],
                                               scale=st[:, 123:124]), reads=[bsrc, bst], writes=list(bhn), c=1024)
            yield
            yield from affine(hn, gB, bB, dst, D, bhn[0], bhn[1], bdst, split)

        b_w16 = [Buf("w16_%d" % i) for i in range(KPRE * 3)]

        def precast(b):
            for hm in ((b - 1) * 2, (b - 1) * 2 + 1):
                if hm >= KPRE * 6:
                    return
                m, hf = hm // 2, hm % 2
                e, mi = m // 3, m % 3
                src_ = (wg_d, wu_d, wd_d)[mi][e].rearrange("(k p) f -> p k f", p=128)
                dst_ = W16_d[m].rearrange("(k p) f -> p k f", p=128)
                P.dma("pool", dst_[:, 4 * hf:4 * hf + 4, :], src_[:, 4 * hf:4 * hf + 4, :], reads=[b_hbuf[b - 1]],
                      accw=[b_w16[m]], nbytes=3145728)

        def blockA(b):
            g = b // 4
            s_cur = (b + 1) % 3
            s_prev = b % 3
            p = b % 2 if b >= 0 else 1
            T = TP[p]
            tb = T["b"]
            st = T["st"]
            bst = tb["st"]
            a0, a1, a2, a3 = 4 * p, 4 * p + 1, 4 * p + 2, 4 * p + 3
            if b >= 0:
                xTb = lambda k: xTg[g % 2][:, k, (b % 4) * 128:(b % 4) * 128 + 128]
                bx = b_xtg[g % 2]
                if b % 4 == 0 and g + 1 < 4:
                    load_group(g + 1)
            else:
                xTb = lambda k: xTh[:, k, :]
                bx = b_xth
            for c in range(2):
                for k in range(8):
                    P.op("pe", lambda e, c=c, k=k: e.matmul(psb(a1)[:, c * 128:(c + 1) * 128],
                                                            lhsT=wkd[:, k, c * 128:(c + 1) * 128], rhs=xTb(k),
                                                            start=(k == 0), stop=(k == 7)),
                         reads=[b_wkd, bx, bank[a1]])
                    yield
            for k in range(8):
                P.op("pe", lambda e, k=k: e.matmul(psb(a1)[:, 256:384], lhsT=xTb(k), rhs=w_in_bf[:, k, 640:768],
                                                   start=(k == 0), stop=False), reads=[b_win, bx, bank[a1]])
                yield
            P.op("pe", lambda e: e.matmul(psb(a1)[:, 256:384], lhsT=ones_row, rhs=btok[0:1, 0:128],
                                          start=False, stop=True), reads=[b_cb, b_small, bank[a1]])
            yield
            for gq in range(2):
                for j in range(2):
                    P.op("act", lambda e, gq=gq, j=j: e.activation(
                        out=KT[64 * j:64 * j + 64, gq, j, s_cur * 128:(s_cur + 1) * 128],
                        in_=psb(a1)[64 * j:64 * j + 64, gq * 128:(gq + 1) * 128], func=AF.Identity,
                        bias=bqk[64 * j:64 * j + 64, 4 + gq:5 + gq], scale=1.0),
                        reads=[bank[a1], b_f], accw=[b_kt[s_cur]])
                    yield
            P.op("act", lambda e: e.activation(out=Vaug[:, s_cur, :, 0:64],
                                               in_=psb(a1)[:, 256:384].rearrange("p (g d) -> p g d", g=2),
                                               func=AF.Copy), reads=[bank[a1]], accw=[b_v[s_cur]])
            yield
            if b < 0:
                return
            xt = T["xtok"]
            P.dma("sp", xt, x_d[b * 128:(b + 1) * 128, :], writes=[tb["xtok"]])
            if b >= 1:
                precast(b)
            for c in range(4):
                for k in range(8):
                    P.op("pe", lambda e, c=c, k=k: e.matmul(psb(a0)[:, c * 128:(c + 1) * 128],
                                                            lhsT=w_in_bf[:, k, c * 128:(c + 1) * 128], rhs=xTb(k),
                                                            start=(k == 0), stop=(k == 7)),
                         reads=[b_win, bx, bank[a0]])
                    yield
            QT = T["QT"]
            for c in range(4):
                P.op("act", lambda e, c=c: e.activation(out=QT[:, c * 128:(c + 1) * 128],
                                                        in_=psb(a0)[:, c * 128:(c + 1) * 128], func=AF.Identity,
                                                        bias=bqk[:, c:c + 1], scale=1.0),
                     reads=[bank[a0], b_f], writes=[tb["QT"]] if c == 0 else (), accw=[tb["QT"]] if c else ())
                yield
            for bi, c0, o0 in [(a2, 768, 128), (a3, 1280, 640)]:
                for k in range(8):
                    P.op("pe", lambda e, bi=bi, c0=c0, k=k: e.matmul(psb(bi), lhsT=xTb(k),
                                                                     rhs=w_in_bf[:, k, c0:c0 + 512],
                                                                     start=(k == 0), stop=False),
                         reads=[b_win, bx, bank[bi]], c=512)
                    yield
                P.op("pe", lambda e, bi=bi, o0=o0: e.matmul(psb(bi), lhsT=ones_row, rhs=btok[0:1, o0:o0 + 512],
                                                            start=False, stop=True),
                     reads=[b_cb, b_small, bank[bi]], c=512)
                yield
            yield from gelu(psb(a2), bank[a2], T["tA"], T["tB"], T["ug"], tb["tA"], tb["tB"], tb["ug"])
            yield from gelu(psb(a3), bank[a3], T["tC"], T["tD"], T["gg"], tb["tC"], tb["tD"], tb["gg"], extra=[tb["tD2"]])
            PT = T["PT"]
            for j in range(2):
                for c in range(4):
                    for kb in range(2):
                        i = c * 2 + kb
                        bi = a0 + i // 4
                        col = (i % 4) * 128
                        sl = s_prev if kb == 0 else s_cur
                        m = (2 if b == 0 else 0) if kb == 0 else 1
                        P.op("pe", lambda e, bi=bi, col=col, c=c, j=j, sl=sl: e.matmul(
                            psb(bi)[:, col:col + 128], lhsT=KT[:, c // 2, j, sl * 128:(sl + 1) * 128],
                            rhs=QT[:, c * 128:(c + 1) * 128], start=True, stop=False),
                            reads=[b_kt[sl], tb["QT"], bank[bi]])
                        yield
                        P.op("pe", lambda e, bi=bi, col=col, m=m: e.matmul(
                            psb(bi)[:, col:col + 128], lhsT=ident, rhs=mkb[:, m * 128:(m + 1) * 128],
                            start=False, stop=True), reads=[b_cb, b_mk, bank[bi]])
                        yield
                P.op("act", lambda e, j=j: e.activation(
                    out=PT[:, j * 1024:(j + 1) * 1024].rearrange("p (a f) -> p a f", a=2),
                    in_=ps[:, a0:a0 + 2, :], func=AF.Exp, scale=0.125), c=1024,
                    reads=[bank[a0], bank[a1]],
                    writes=[tb["PT"]] if j == 0 else (), accw=[tb["PT"]] if j else ())
                yield
            gg, gnf, tC, tD = T["gg"], T["gnf"], T["tC"], T["tD"]
            stv = lambda a, n=8: st[:, a:a + n]
            gg3 = gg.rearrange("p (g d) -> p g d", g=8)
            P.op("dve", lambda e: e.tensor_reduce(out=stv(0), in_=gg3, axis=AX.X, op=ALU.add),
                 reads=[tb["gg"]], writes=[bst], c=512)
            yield
            P.op("act", lambda e: e.activation(out=tC, in_=gg, func=AF.Square), reads=[tb["gg"]],
                 writes=[tb["tC"]])
            yield
            P.op("dve", lambda e: e.tensor_reduce(out=stv(8), in_=tC.rearrange("p (g d) -> p g d", g=8),
                                                  axis=AX.X, op=ALU.add), reads=[tb["tC"]], writes=[bst], c=512)
            yield
            P.op("dve", lambda e: e.tensor_scalar(out=stv(16), in0=stv(0), scalar1=1.0 / 64, scalar2=None,
                                                  op0=ALU.mult), reads=[bst], writes=[bst])
            yield
            P.op("dve", lambda e: e.tensor_tensor(out=stv(24), in0=stv(16), in1=stv(16), op=ALU.mult),
                 reads=[bst], writes=[bst])
            yield
            P.op("dve", lambda e: e.scalar_tensor_tensor(out=stv(32), in0=stv(8), scalar=1.0 / 64, in1=stv(24),
                                                         op0=ALU.mult, op1=ALU.subtract), reads=[bst], writes=[bst])
            yield
            yield from ln_tail(stv(16), stv(32), stv(40), stv(48), stv(56), bst, 8, 4.0 * EPS)
            for gi in range(8):
                P.op("dve", lambda e, gi=gi: e.tensor_scalar(out=gnf[:, gi * 64:(gi + 1) * 64],
                                                             in0=gg[:, gi * 64:(gi + 1) * 64],
                                                             scalar1=st[:, 48 + gi:49 + gi],
                                                             scalar2=st[:, 56 + gi:57 + gi],
                                                             op0=ALU.mult, op1=ALU.add),
                     reads=[tb["gg"], bst], writes=[tb["gnf"]] if gi == 0 else (),
                     accw=[tb["gnf"]] if gi else (), c=64)
                yield
            gn_bf = T["gn"]
            for eng, lo, hi, bb, kk in (("dve", 0, 320, tb["tD"], 2), ("pool", 320, 512, tb["tD2"], 1)):
                P.op(eng, lambda e, lo=lo, hi=hi: e.tensor_tensor(out=tD[:, lo:hi], in0=gnf[:, lo:hi],
                                                                  in1=lvgB[:, lo:hi], op=ALU.mult),
                     reads=[tb["gnf"], b_f], writes=[bb], c=kk * (hi - lo))
                yield
            for eng, lo, hi, bb, kk in (("dve", 0, 320, tb["tD"], 2), ("pool", 320, 512, tb["tD2"], 1)):
                P.op(eng, lambda e, lo=lo, hi=hi: e.tensor_tensor(out=gn_bf[:, lo:hi], in0=tD[:, lo:hi],
                                                                  in1=lvbB[:, lo:hi], op=ALU.add),
                     reads=[bb, b_f], accw=[tb["gn"]], c=kk * (hi - lo))
                yield
            for h in range(8):
                c, j = h // 2, h % 2
                gq = c // 2
                bi = a0 + h // 4
                hh = h % 4
                for kb in range(2):
                    sl = s_prev if kb == 0 else s_cur
                    i = c * 2 + kb
                    P.op("pe", lambda e, bi=bi, hh=hh, j=j, i=i, sl=sl, gq=gq, kb=kb: e.matmul(
                        psb(bi)[:, hh * 65:(hh + 1) * 65], lhsT=PT[:, j * 1024 + i * 128:j * 1024 + (i + 1) * 128],
                        rhs=Vaug[:, sl, gq, :], start=(kb == 0), stop=(kb == 1)),
                        reads=[tb["PT"], b_v[sl], bank[bi]])
                    yield
            attn_bf = T["attn"]
            for hf in range(2):
                Ov = psb(a0 + hf)[:, 0:260].rearrange("p (h d) -> p h d", h=4)
                P.op("dve", lambda e, Ov=Ov, hf=hf: e.tensor_tensor(
                    out=st[:, 64 + 4 * hf:68 + 4 * hf].unsqueeze(2), in0=Ov[:, :, 64:65],
                    in1=esink[:, 4 * hf:4 * hf + 4].unsqueeze(2), op=ALU.add),
                    reads=[bank[a0 + hf], b_f], writes=[bst])
                yield
                P.op("dve", lambda e, hf=hf: e.reciprocal(out=st[:, 72 + 4 * hf:76 + 4 * hf],
                                                          in_=st[:, 64 + 4 * hf:68 + 4 * hf]),
                     reads=[bst], writes=[bst])
                yield
                P.op("dve", lambda e, Ov=Ov, hf=hf: e.tensor_tensor(
                    out=attn_bf[:, hf * 256:(hf + 1) * 256].rearrange("p (h d) -> p h d", h=4),
                    in0=Ov[:, :, 0:64],
                    in1=st[:, 72 + 4 * hf:76 + 4 * hf].unsqueeze(2).to_broadcast([128, 4, 64]), op=ALU.mult),
                    reads=[bank[a0 + hf], bst], writes=[tb["attn"]] if hf == 0 else (),
                    accw=[tb["attn"]] if hf else (), c=256)
                yield
            for gi in range(8):
                P.op("pe", lambda e, gi=gi: e.matmul(psb(a2)[:, gi * 64:(gi + 1) * 64], lhsT=wsTm[:, gi, :],
                                                     rhs=gn_bf[:, gi * 64:(gi + 1) * 64], start=True, stop=True),
                     reads=[b_ws, tb["gn"], bank[a2]], c=64)
                yield
            sgu_bf, ug = T["sgu"], T["ug"]
            for gi in range(8):
                P.op("dve", lambda e, gi=gi: e.scalar_tensor_tensor(
                    out=sgu_bf[:, gi * 64:(gi + 1) * 64], in0=psb(a2)[:, gi * 64:(gi + 1) * 64],
                    scalar=bsT[:, gi:gi + 1], in1=ug[:, gi * 64:(gi + 1) * 64], op0=ALU.add, op1=ALU.mult),
                    reads=[bank[a2], b_f, tb["ug"]], writes=[tb["sgu"]] if gi == 0 else (),
                    accw=[tb["sgu"]] if gi else (), c=64)
                yield
            catT = T["catT"]
            for i in range(8):
                src = attn_bf if i < 4 else sgu_bf
                bsrc = tb["attn"] if i < 4 else tb["sgu"]
                ii = i % 4
                P.op("pe", lambda e, i=i, ii=ii, src=src: e.transpose(
                    out=psbf(a3)[:, i * 128:(i + 1) * 128], in_=src[:, ii * 128:(ii + 1) * 128], identity=ident),
                    reads=[bsrc, b_cb, bank[a3]])
                yield
            P.op("act", lambda e: e.activation(out=catT, in_=psbf(a3), func=AF.Copy), reads=[bank[a3]],
                 writes=[tb["catT"]], c=1024)
            yield
            zb = T["z"]
            for hf in range(2):
                for fc in range(8):
                    P.op("pe", lambda e, hf=hf, fc=fc: e.matmul(psb(a0 + hf), lhsT=catT[:, fc * 128:(fc + 1) * 128],
                                                                rhs=w_out_bf[:, fc, hf * 512:(hf + 1) * 512],
                                                                start=(fc == 0), stop=False),
                         reads=[tb["catT"], b_wout, bank[a0 + hf]], c=512)
                    yield
                P.op("pe", lambda e, hf=hf: e.matmul(psb(a0 + hf), lhsT=ones_row,
                                                     rhs=boutb[0:1, hf * 512:(hf + 1) * 512], start=False, stop=True),
                     reads=[b_cb, b_small, bank[a0 + hf]], c=512)
                yield
                P.op("dve", lambda e, hf=hf: e.scalar_tensor_tensor(
                    out=zb[:, hf * 512:(hf + 1) * 512], in0=xt[:, hf * 512:(hf + 1) * 512], scalar=ALPHA,
                    in1=psb(a0 + hf), op0=ALU.mult, op1=ALU.add),
                    reads=[tb["xtok"], bank[a0 + hf]], writes=[tb["z"]] if hf == 0 else (),
                    accw=[tb["z"]] if hf else (), c=512)
                yield
            h, hb = T["h"], hb4[b % 4]
            bhb = b_hb4[b % 4]
            yield from layer_norm(zb, tb["z"], l1gB, l1bB, h, tb["h"], T["hn"], (tb["hn"], tb["hn2"]), st, bst)
            P.dma("sp", hbuf_d[b * 128:(b + 1) * 128, :], h, reads=[tb["h"]], writes=[b_hbuf[b]])
            P.op("act", lambda e: e.activation(out=hb, in_=h, func=AF.Copy), reads=[tb["h"]], writes=[bhb],
                 c=1024)
            yield
            hT = T["hT"]
            for k in range(8):
                P.op("pe", lambda e, k=k: e.transpose(out=psbf(a2)[:, k * 128:(k + 1) * 128],
                                                      in_=hb[:, k * 128:(k + 1) * 128], identity=ident),
                     reads=[bhb, b_cb, bank[a2]])
                yield
            P.op("act", lambda e: e.activation(out=hT, in_=psbf(a2), func=AF.Copy), reads=[bank[a2]],
                 writes=[tb["hT"]], c=1024)
            yield
            for k in range(8):
                P.op("pe", lambda e, k=k: e.matmul(psb(a3)[:, 0:32], lhsT=hT[:, k * 128:(k + 1) * 128],
                                                   rhs=wr_bf[:, k, :], start=(k == 0), stop=False),
                     reads=[tb["hT"], b_wr, bank[a3]])
                yield
            P.op("pe", lambda e: e.matmul(psb(a3)[:, 0:32], lhsT=ones_row, rhs=brb[0:1, :], start=False, stop=True),
                 reads=[b_cb, b_small, bank[a3]])
            yield
            lg, junk, fl, slotv, maskb = T["lg"], T["junk"], T["fl"], T["slotv"], T["maskb"]
            P.op("dve", lambda e: e.tensor_copy(out=lg, in_=psb(a3)[:, 0:32]), reads=[bank[a3]], writes=[tb["lg"]])
            yield
            P.op("dve", lambda e: e.max(out=stv(80), in_=lg), reads=[tb["lg"]], writes=[bst])
            yield
            P.op("dve", lambda e: e.tensor_scalar(out=maskb, in0=lg, scalar1=st[:, 83:84], scalar2=None,
                                                  op0=ALU.is_ge), reads=[tb["lg"], bst], writes=[tb["maskb"]])
            yield
            P.op("pe", lambda e: e.matmul(psb(a3)[:, 32:64], lhsT=lstrict, rhs=maskb, start=True, stop=True),
                 reads=[b_cb, tb["maskb"], bank[a3]])
            yield
            P.op("pe", lambda e: e.matmul(psb(a3)[:, 64:96], lhsT=ones_m, rhs=maskb, start=True, stop=True),
                 reads=[b_cb, tb["maskb"], bank[a3]])
            yield
            P.op("dve", lambda e: e.tensor_tensor(out=slotv, in0=psb(a3)[:, 32:64], in1=base[:], op=ALU.add),
                 reads=[bank[a3], b_base], writes=[tb["slotv"]])
            yield
            P.op("dve", lambda e: e.tensor_tensor(out=base[:], in0=psb(a3)[:, 64:96], in1=base[:], op=ALU.add),
                 reads=[bank[a3]], writes=[b_base])
            yield
            P.op("dve", lambda e: e.tensor_scalar(out=fl, in0=slotv, scalar1=float(CAP), scalar2=1.0e6,
                                                  op0=ALU.is_ge, op1=ALU.mult),
                 reads=[tb["slotv"]], writes=[tb["fl"]])
            yield
            P.op("dve", lambda e: e.tensor_tensor(out=fl, in0=fl, in1=slotv, op=ALU.add),
                 reads=[tb["slotv"]], writes=[tb["fl"]])
            yield
            P.op("dve", lambda e: e.tensor_tensor(out=fl, in0=fl, in1=eC[:], op=ALU.add),
                 reads=[b_f], writes=[tb["fl"]])
            yield
            for j in range(4):
                P.op("dve", lambda e, j=j: e.scalar_tensor_tensor(
                    out=junk, in0=lg, scalar=st[:, 80 + j:81 + j], in1=fl, op0=ALU.is_equal, op1=ALU.mult,
                    accum_out=st[:, 96 + j:97 + j]), reads=[tb["lg"], tb["fl"], bst],
                    writes=[tb["junk"], bst])
                yield
            P.op("dve", lambda e: e.tensor_copy(out=idx_all[:, b * 4:(b + 1) * 4], in_=st[:, 96:100]),
                 reads=[bst], accw=[b_idx])
            yield
            P.op("dve", lambda e: e.tensor_scalar(out=st[:, 100:101], in0=st[:, 80:81], scalar1=-1.0,
                                                  scalar2=None, op0=ALU.mult), reads=[bst], writes=[bst])
            yield
            P.op("act", lambda e: e.activation(out=st[:, 104:108], in_=st[:, 80:84], func=AF.Exp,
                                               bias=st[:, 100:101], scale=1.0, accum_out=st[:, 101:102]),
                 reads=[bst], writes=[bst])
            yield
            P.op("dve", lambda e: e.reciprocal(out=st[:, 102:103], in_=st[:, 101:102]), reads=[bst], writes=[bst])
            yield
            P.op("dve", lambda e: e.tensor_scalar(out=st[:, 104:108], in0=st[:, 104:108],
                                                  scalar1=st[:, 102:103], scalar2=None, op0=ALU.mult),
                 reads=[bst], writes=[bst])
            yield
            P.op("dve", lambda e: e.scalar_tensor_tensor(out=gate_all[:, b * 4:(b + 1) * 4], in0=st[:, 96:100],
                                                         scalar=float(NE * CAP), in1=st[:, 104:108],
                                                         op0=ALU.is_lt, op1=ALU.mult),
                 reads=[bst], accw=[b_gate])
            yield
            for j in range(4):
                P.op("pool", lambda e, j=j: e.indirect_dma_start(
                    out=Xg_d[:, :], out_offset=bass.IndirectOffsetOnAxis(ap=idx_all[:, b * 4 + j:b * 4 + j + 1], axis=0),
                    in_=hb, in_offset=None, bounds_check=breg(e), oob_is_err=False),
                    reads=[bhb, b_idx], accw=[b_Xg], dma=True, nbytes=262144)
                yield

        for _ in blockA(-1):
            pass
        run_pipeline([(lambda b=b: blockA(b)) for b in range(NB)], lag=0, maxflight=1)
        P.dma("sp", cnt_d, base[:], reads=[b_base])

        A.reset()
        B.reset()
        wring = [A.bf16(8 * D).rearrange("p (k f) -> p k f", k=8) for _ in range(6)]
        b_wring = [Buf("wring%d" % i) for i in range(6)]
        Xe = [B.bf16(CT * D).rearrange("p (r d) -> p r d", r=CT) for _ in range(2)]
        XeT = [B.bf16(8 * CAP).rearrange("p (k c) -> p k c", k=8) for _ in range(2)]
        hidT = [B.bf16(8 * CAP).rearrange("p (k c) -> p k c", k=8) for _ in range(2)]
        gts = [B.f32(CAP) for _ in range(2)]
        sgs = [B.f32(CAP) for _ in range(2)]
        u1s = [B.f32(CAP) for _ in range(2)]
        gss = [B.f32(CAP) for _ in range(2)]
        ysb = [B.f32(D) for _ in range(2)]
        b_Xe, b_XeT, b_hid = ([Buf(n + str(i)) for i in range(2)] for n in ("Xe", "XeT", "hidT"))
        b_gt, b_sg, b_u1, b_gs = ([Buf(n + str(i)) for i in range(2)] for n in ("gt", "sg", "u1", "gs"))
        b_ysb = [Buf("ysb0"), Buf("ysb1")]
        b_bdB = [Buf("bdB0"), Buf("bdB1")]
        P.barrier(b_wring[4:] + b_Xe + b_XeT + b_hid + b_gt + b_sg + b_u1 + b_gs + b_ysb + b_bdB)
        for slot, olds in ((0, [b_win]), (1, [b_win, b_wkd]), (2, [b_wout]), (3, b_xtg)):
            for ob in olds:
                b_wring[slot].r |= ob.r | ob.aw
                if ob.w is not None:
                    b_wring[slot].r.add(ob.w)

        def load_w(e):
            for mi, wd_ in enumerate((wg_d, wu_d, wd_d)):
                slot = (e % 2) * 3 + mi
                if e < KPRE:
                    wr_ = W16_d[e * 3 + mi].rearrange("(k p) f -> p k f", p=128)
                    rd_, nb_ = [b_w16[e * 3 + mi]], 1048576
                else:
                    wr_ = wd_[e].rearrange("(k p) f -> p k f", p=128)
                    rd_, nb_ = [], 2097152
                for hf in range(2):
                    P.dma("pool", wring[slot][:, 4 * hf:4 * hf + 4, :], wr_[:, 4 * hf:4 * hf + 4, :],
                          reads=rd_, accw=[b_wring[slot]], nbytes=nb_, single_packet=True)
            P.dma("pool", bdrow[e % 2][:], bd_d[e:e + 1, :], writes=[b_bdB[e % 2]], nbytes=4096)

        def front(e):
            q2 = e % 2
            P.dma("sp", Xe[q2][:], Xg_d[e * CAP:(e + 1) * CAP, :].rearrange("(r p) d -> p r d", p=128),
                  reads=[b_Xg], writes=[b_Xe[q2]], nbytes=CAP * 2048)

        def front_pe(e):
            q2 = e % 2
            for kp in range(4):
                bi = kp % 2
                for kk in range(2):
                    k = 2 * kp + kk
                    for r in range(CT):
                        P.op("pe", lambda e_, bi=bi, kk=kk, r=r, k=k: e_.transpose(
                            out=psbf(bi)[:, kk * CAP + r * 128:kk * CAP + (r + 1) * 128],
                            in_=Xe[q2][:, r, k * 128:(k + 1) * 128], identity=ident),
                            reads=[b_Xe[q2], b_cb, bank[bi]])
                P.op("act", lambda e_, bi=bi, kp=kp: e_.activation(
                    out=XeT[q2][:, 2 * kp:2 * kp + 2, :],
                    in_=psbf(bi)[:, 0:2 * CAP].rearrange("p (k c) -> p k c", k=2),
                    func=AF.Copy), reads=[bank[bi]], writes=[b_XeT[q2]] if kp == 0 else (),
                    accw=[b_XeT[q2]] if kp else (), c=2 * CAP)

        def gate_up(e):
            q2 = e % 2
            wg_s, wu_s = wring[q2 * 3], wring[q2 * 3 + 1]
            bwg, bwu = b_wring[q2 * 3], b_wring[q2 * 3 + 1]
            for fc in range(8):
                pb = 2 + 2 * (fc % 2)
                for wi, (w_s, bw) in enumerate(((wg_s, bwg), (wu_s, bwu))):
                    for k in range(8):
                        P.op("pe", lambda e_, pb=pb, wi=wi, w_s=w_s, k=k, fc=fc: e_.matmul(
                            psb(pb + wi)[:, 0:CAP], lhsT=w_s[:, k, fc * 128:(fc + 1) * 128], rhs=XeT[q2][:, k, :],
                            start=(k == 0), stop=(k == 7)), reads=[bw, b_XeT[q2], bank[pb + wi]], c=CAP)
                q = fc % 2
                col = e * 8 + fc
                P.op("dve", lambda e_, pb=pb, q=q, col=col: e_.tensor_scalar(
                    out=gts[q], in0=psb(pb)[:, 0:CAP], scalar1=bgT[:, col:col + 1], scalar2=7.0,
                    op0=ALU.add, op1=ALU.min), reads=[bank[pb], b_f], writes=[b_gt[q]], c=CAP)
                P.op("act", lambda e_, q=q: e_.activation(out=sgs[q], in_=gts[q], func=AF.Sigmoid, scale=1.702),
                     reads=[b_gt[q]], writes=[b_sg[q]], c=CAP)
                P.op("dve", lambda e_, pb=pb, q=q, col=col: e_.tensor_scalar(
                    out=u1s[q], in0=psb(pb + 1)[:, 0:CAP], scalar1=bu1[:, col:col + 1], scalar2=8.0,
                    op0=ALU.add, op1=ALU.min), reads=[bank[pb + 1], b_f], writes=[b_u1[q]], c=CAP)
                P.op("pool", lambda e_, q=q: e_.tensor_tensor(out=gss[q], in0=gts[q], in1=sgs[q], op=ALU.mult),
                     reads=[b_gt[q], b_sg[q]], writes=[b_gs[q]], c=CAP)
                P.op("dve", lambda e_, q=q, fc=fc: e_.scalar_tensor_tensor(
                    out=hidT[q2][:, fc, :], in0=u1s[q], scalar=-6.0, in1=gss[q], op0=ALU.max, op1=ALU.mult),
                    reads=[b_u1[q], b_gs[q]], writes=[b_hid[q2]] if fc == 0 else (),
                    accw=[b_hid[q2]] if fc else (), c=2 * CAP)

        def down(e):
            q2 = e % 2
            wd_s, bwd = wring[q2 * 3 + 2], b_wring[q2 * 3 + 2]
            for r in range(CT):
                yb = ysb[r % 2]
                for hf in range(2):
                    for fc in range(8):
                        P.op("pe", lambda e_, hf=hf, fc=fc, r=r: e_.matmul(
                            psb(6 + hf), lhsT=hidT[q2][:, fc, r * 128:(r + 1) * 128],
                            rhs=wd_s[:, fc, hf * 512:(hf + 1) * 512], start=(fc == 0), stop=False),
                            reads=[b_hid[q2], bwd, bank[6 + hf]], c=512)
                    P.op("pe", lambda e_, hf=hf: e_.matmul(
                        psb(6 + hf), lhsT=ones_row, rhs=bdrow[q2][0:1, hf * 512:(hf + 1) * 512],
                        start=False, stop=True), reads=[b_cb, b_bdB[q2], bank[6 + hf]], c=512)
                    P.op("act", lambda e_, hf=hf, yb=yb: e_.activation(
                        out=yb[:, hf * 512:(hf + 1) * 512], in_=psb(6 + hf), func=AF.Copy),
                        reads=[bank[6 + hf]], writes=[b_ysb[r % 2]] if hf == 0 else (),
                        accw=[b_ysb[r % 2]] if hf else (), c=512)
                P.dma("sp", Y_d[e * CAP + r * 128:e * CAP + (r + 1) * 128, :], yb, reads=[b_ysb[r % 2]],
                      accw=[b_Y], nbytes=524288)

        load_w(0)
        front(0)
        front_pe(0)
        for e in range(NE):
            if e + 1 < NE:
                load_w(e + 1)
                front(e + 1)
            gate_up(e)
            if e + 1 < NE:
                front_pe(e + 1)
            down(e)

        A.reset()
        NC3 = 3
        Yg = [[A.f32(D) for _ in range(4)] for _ in range(NC3)]
        hC = [A.f32(D) for _ in range(NC3)]
        acc = [A.f32(D) for _ in range(NC3)]
        yo = [A.f32(D) for _ in range(NC3)]
        hnC = [A.f32(D) for _ in range(NC3)]
        b_Yg = [[Buf("Yg%d%d" % (s, j)) for j in range(4)] for s in range(NC3)]
        b_hC = [Buf("hC%d" % i) for i in range(NC3)]
        b_acc = [Buf("acc%d" % i) for i in range(NC3)]
        b_yo = [Buf("yo%d" % i) for i in range(NC3)]
        b_hnC = [Buf("hnC%d" % i) for i in range(NC3)]
        b_hnC2 = [Buf("hnCb%d" % i) for i in range(NC3)]
        b_stC = [Buf("stC%d" % i) for i in range(NC3)]
        P.barrier([x for s in b_Yg for x in s] + b_hC + b_acc + b_yo + b_hnC + b_hnC2 + b_stC)

        for s_i in range(NC3):
            for j_i in range(4):
                P.op("dve" if j_i % 2 == 0 else "pool", lambda e, s_i=s_i, j_i=j_i: e.memset(Yg[s_i][j_i], 0.0),
                     writes=[b_Yg[s_i][j_i]], c=1024)

        def blockC(b):
            s = b % NC3
            P.dma("sp", hC[s], hbuf_d[b * 128:(b + 1) * 128, :], reads=[b_hbuf[b]], writes=[b_hC[s]], nbytes=524288)
            for j in range(4):
                P.op("pool", lambda e, j=j: e.indirect_dma_start(
                    out=Yg[s][j], out_offset=None, in_=Y_d[:, :],
                    in_offset=bass.IndirectOffsetOnAxis(ap=idx_all[:, b * 4 + j:b * 4 + j + 1], axis=0),
                    bounds_check=breg(e), oob_is_err=False),
                    reads=[b_Y, b_idx], writes=[b_Yg[s][j]], dma=True, nbytes=524288)
                yield
            P.op("act", lambda e: e.activation(out=acc[s], in_=hC[s], func=AF.Copy, scale=ALPHA),
                 reads=[b_hC[s]], writes=[b_acc[s]], c=1024)
            yield
            for j in range(4):
                P.op("dve", lambda e, j=j: e.scalar_tensor_tensor(
                    out=acc[s], in0=Yg[s][j], scalar=gate_all[:, b * 4 + j:b * 4 + j + 1], in1=acc[s],
                    op0=ALU.mult, op1=ALU.add), reads=[b_Yg[s][j], b_gate], writes=[b_acc[s]], c=1024)
                yield
            yield from layer_norm(acc[s], b_acc[s], l2gB, l2bB, yo[s], b_yo[s], hnC[s], (b_hnC[s], b_hnC2[s]), stC[s], b_stC[s],
                                  act_stats=True, split=512)
            tk = P.dma("sp", y_d[b * 128:(b + 1) * 128, :], yo[s], reads=[b_yo[s]], nbytes=524288)
            P.final.append(tk)
            yield

        run_pipeline([(lambda b=b: blockC(b)) for b in range(NB)], lag=0, maxflight=1)

        P.emit(block)
    return nc


_NC_CACHE = {}


def _prep_inputs(inp):
    f = lambda a: np.ascontiguousarray(np.asarray(a, dtype=np.float32))
    x = f(inp["x"])
    w_in = f(inp["w_in"])[0]
    b_in = f(inp["b_in"])[0]
    p = np.arange(128)
    bqk = np.zeros((128, 6), np.float32)
    for c in range(4):
        bqk[:, c] = b_in[c * 128 + p]
    for g in range(2):
        bqk[:, 4 + g] = b_in[512 + g * 64 + (p % 64)]
    s_i = np.arange(128)[:, None]
    t_i = np.arange(128)[None, :]
    mk_prev = np.where(s_i > t_i, 0.0, NEG).astype(np.float32)
    mk_cur = np.where(s_i <= t_i, 0.0, NEG).astype(np.float32)
    mk_none = np.full((128, 128), NEG, np.float32)
    cst = np.concatenate([np.eye(128, dtype=np.float32), (s_i < t_i).astype(np.float32),
                          (s_i <= t_i).astype(np.float32), np.ones((128, 128), np.float32)], axis=1)
    cf = np.tile((np.arange(NE, dtype=np.float32) * CAP)[None, :], (128, 1))
    shared = {
        "cst": cst, "cf": np.ascontiguousarray(cf),
        "w_in": w_in, "bqk": bqk, "btok": np.ascontiguousarray(b_in[None, 640:1792]),
        "snk": f(inp["sinks"]).reshape(1, 8),
        "lvg": f(inp["ln_v_g"]).reshape(1, 512), "lvb": f(inp["ln_v_b"]).reshape(1, 512),
        "wsT": np.ascontiguousarray(f(inp["w_spatial"])[0].transpose(0, 2, 1)),
        "bsT": np.ascontiguousarray(f(inp["b_spatial"])[0].T),
        "w_out": f(inp["w_out"])[0], "b_out": f(inp["b_out"]).reshape(1, D),
        "l1g": f(inp["ln1_g"]).reshape(1, D), "l1b": f(inp["ln1_b"]).reshape(1, D),
        "l2g": f(inp["ln2_g"]).reshape(1, D), "l2b": f(inp["ln2_b"]).reshape(1, D),
        "wr": f(inp["w_router"])[0], "br": f(inp["b_router"]).reshape(1, NE),
        "wg": f(inp["w_gate"])[0], "wu": f(inp["w_up"])[0], "wd": f(inp["w_down"])[0],
        "bgT": np.ascontiguousarray(f(inp["b_gate"])[0].reshape(NE, 8, 128).transpose(2, 0, 1).reshape(128, 256)),
        "buT": np.ascontiguousarray(f(inp["b_up"])[0].reshape(NE, 8, 128).transpose(2, 0, 1).reshape(128, 256)),
        "bd": f(inp["b_down"])[0],
    }
    maps = []
    for c in range(NCORES):
        bi, q = c // 4, c % 4
        t0 = q * TOK
        xs = x[bi, t0:t0 + TOK]
        halo = x[bi, t0 - 128:t0] if q > 0 else np.zeros((128, D), np.float32)
        xT = np.ascontiguousarray(np.concatenate([halo, xs], axis=0).T)
        mk = np.concatenate([mk_prev, mk_cur, mk_prev if q > 0 else mk_none], axis=1)
        m = dict(shared)
        m["xT"] = xT
        m["x"] = np.ascontiguousarray(xs)
        m["mk"] = np.ascontiguousarray(mk)
        maps.append(m)
    return maps


def kernel(**inputs):
    if "nc" not in _NC_CACHE:
        _NC_CACHE["nc"] = build_nc()
    nc = _NC_CACHE["nc"]
    maps = _prep_inputs(inputs)
    res = run_bass_kernel_spmd(nc, maps, core_ids=list(range(NCORES)))
    out = np.empty((2, 8192, D), np.float32)
    _NC_CACHE["cnt"] = [np.asarray(res.results[c]["cnt"])[0] for c in range(NCORES)]
    for c in range(NCORES):
        out[c // 4, (c % 4) * TOK:(c % 4 + 1) * TOK] = np.asarray(res.results[c]["y"], dtype=np.float32)
    return out
```
